# Optimizing a Trainium2 kernel written in Bass

```python
import jax, jax.numpy as jnp
from jax import lax
import numpy as np

D_MODEL = 2048
BATCH = 4
SEQ = 4096
DEPTH = 1

CHUNK = 64
Q_BLOCK = 128
EPS = 1e-6

MLA_HEADS = 8
MLA_Q_RANK = 512
MLA_KV_RANK = 256
MLA_NOPE = 128
MLA_ROPE = 64
MLA_V = 128
ROPE_THETA = 10000.0

SB_HEADS = 8
SB_DIM = 128
SB_WIDTH = SB_HEADS * SB_DIM

PEER_HEADS = 8
PEER_NKEYS = 128
PEER_N = PEER_NKEYS * PEER_NKEYS
PEER_DKEY = 256
PEER_TOPK = 16
PEER_TOK_BLOCK = 128

IN_SIZES = (MLA_Q_RANK, MLA_KV_RANK, MLA_ROPE, SB_WIDTH, SB_WIDTH, SB_WIDTH, D_MODEL, D_MODEL)
IN_COLS = MLA_Q_RANK + MLA_KV_RANK + MLA_ROPE + 3 * SB_WIDTH + 2 * D_MODEL
N_ADA = 6

kernel_name = "hybrid_mla_stickbreak_peer_block"


def rmsnorm(x, g):
    xf = x.astype(jnp.float32)
    y = xf * lax.rsqrt(jnp.mean(xf * xf, axis=-1, keepdims=True) + EPS)
    return (y * g.astype(jnp.float32)).astype(x.dtype)


def rope(x, cos, sin):
    half = x.shape[-1] // 2
    x1, x2 = x[..., :half], x[..., half:]
    return jnp.concatenate([x1 * cos - x2 * sin, x2 * cos + x1 * sin], axis=-1)


def mla_attention(q_nope, q_rope, k_nope, k_rope, v):
    B, S, H, _ = q_nope.shape
    nb = S // Q_BLOCK
    qn = q_nope.reshape(B, nb, Q_BLOCK, H, MLA_NOPE).transpose(1, 0, 2, 3, 4)
    qr = q_rope.reshape(B, nb, Q_BLOCK, H, MLA_ROPE).transpose(1, 0, 2, 3, 4)
    q0s = jnp.arange(nb, dtype=jnp.int32) * Q_BLOCK
    key_chunk = jnp.arange(S, dtype=jnp.int32) // CHUNK
    scale = (MLA_NOPE + MLA_ROPE) ** -0.5

    def block(args):
        qn_b, qr_b, q0 = args
        s = (jnp.einsum('bqhd,bkhd->bhqk', qn_b, k_nope)
             + jnp.einsum('bqhd,bkd->bhqk', qr_b, k_rope)).astype(jnp.float32) * scale
        q_chunk = (q0 + jnp.arange(Q_BLOCK, dtype=jnp.int32)) // CHUNK
        allowed = key_chunk[None, :] <= q_chunk[:, None]
        s = jnp.where(allowed, s, -jnp.inf)
        p = jax.nn.softmax(s, axis=-1).astype(v.dtype)
        return jnp.einsum('bhqk,bkhd->bqhd', p, v)

    out = lax.map(block, (qn, qr, q0s))
    return out.transpose(1, 0, 2, 3, 4).reshape(B, S, H * MLA_V)


def stick_breaking_attention(q, k, v):
    B, S, H, Dh = q.shape
    nb = S // Q_BLOCK
    qb = q.reshape(B, nb, Q_BLOCK, H, Dh).transpose(1, 0, 2, 3, 4)
    q0s = jnp.arange(nb, dtype=jnp.int32) * Q_BLOCK
    key_pos = jnp.arange(S, dtype=jnp.int32)
    scale = Dh ** -0.5

    def block(args):
        q_b, q0 = args
        z = jnp.einsum('bqhd,bkhd->bhqk', q_b, k).astype(jnp.float32) * scale
        q_pos = q0 + jnp.arange(Q_BLOCK, dtype=jnp.int32)
        strict = key_pos[None, :] < q_pos[:, None]
        log_not = jnp.where(strict, jax.nn.log_sigmoid(-z), 0.0)
        later = lax.cumsum(log_not, axis=3, reverse=True) - log_not
        w = jnp.where(strict, jnp.exp(jax.nn.log_sigmoid(z) + later), 0.0)
        return jnp.einsum('bhqk,bkhd->bqhd', w.astype(v.dtype), v)

    out = lax.map(block, (qb, q0s))
    return out.transpose(1, 0, 2, 3, 4).reshape(B, S, H * Dh)


def token_mixer(h, cos, sin, w_in, q_norm_g, kv_norm_g, w_uq, w_ukv, w_branch_mla, w_branch_sb, w_out):
    B, S, _ = h.shape
    proj = h @ w_in
    cq, ckv, kr, sbq, sbk, sbv, gate_a, gate_b = jnp.split(proj, np.cumsum(IN_SIZES)[:-1].tolist(), axis=-1)
    q = (rmsnorm(cq, q_norm_g) @ w_uq).reshape(B, S, MLA_HEADS, MLA_NOPE + MLA_ROPE)
    q_nope, q_rope = q[..., :MLA_NOPE], q[..., MLA_NOPE:]
    q_rope = rope(q_rope, cos[:, :, None, :], sin[:, :, None, :])
    kv = (rmsnorm(ckv, kv_norm_g) @ w_ukv).reshape(B, S, MLA_HEADS, MLA_NOPE + MLA_V)
    k_nope, v_mla = kv[..., :MLA_NOPE], kv[..., MLA_NOPE:]
    k_rope = rope(kr, cos, sin)
    y_a = mla_attention(q_nope, q_rope, k_nope, k_rope, v_mla) @ w_branch_mla
    shp = (B, S, SB_HEADS, SB_DIM)
    y_b = stick_breaking_attention(sbq.reshape(shp), sbk.reshape(shp), sbv.reshape(shp)) @ w_branch_sb
    merged = jax.nn.sigmoid(gate_a) * y_a + jax.nn.sigmoid(gate_b) * y_b
    return merged @ w_out


def peer_ffn(h, w_q, sub_keys, u_tab, v_tab):
    B, S, D = h.shape
    q = (h @ w_q).reshape(B, S, PEER_HEADS, PEER_DKEY).astype(jnp.float32)
    half = PEER_DKEY // 2
    s1 = jnp.einsum('bshd,hnd->bshn', q[..., :half], sub_keys[:, 0].astype(jnp.float32))
    s2 = jnp.einsum('bshd,hnd->bshn', q[..., half:], sub_keys[:, 1].astype(jnp.float32))
    v1, i1 = lax.top_k(s1, PEER_TOPK)
    v2, i2 = lax.top_k(s2, PEER_TOPK)
    cand = (v1[..., :, None] + v2[..., None, :]).reshape(B, S, PEER_HEADS, PEER_TOPK * PEER_TOPK)
    sc, ci = lax.top_k(cand, PEER_TOPK)
    e1 = jnp.take_along_axis(i1, ci // PEER_TOPK, axis=-1)
    e2 = jnp.take_along_axis(i2, ci % PEER_TOPK, axis=-1)
    idx = e1 * PEER_NKEYS + e2
    g = jax.nn.softmax(sc, axis=-1).astype(h.dtype)
    nblk = (B * S) // PEER_TOK_BLOCK
    hb = h.reshape(nblk, PEER_TOK_BLOCK, D)
    ib = idx.reshape(nblk, PEER_TOK_BLOCK, PEER_HEADS * PEER_TOPK)
    gb = g.reshape(nblk, PEER_TOK_BLOCK, PEER_HEADS * PEER_TOPK)

    def block(args):
        h_b, i_b, g_b = args
        act = jax.nn.gelu(jnp.einsum('td,tkd->tk', h_b, u_tab[i_b]))
        return jnp.einsum('tk,tkd->td', g_b * act, v_tab[i_b])

    out = lax.map(block, (hb, ib, gb))
    return out.reshape(B, S, D)


def setup_inputs(seed: int = 0) -> dict:
    key = jax.random.key(seed)
    ks = jax.random.split(key, 20)

    def nrm(k, shape, scale):
        return jax.random.normal(k, shape, jnp.float32) * scale

    d_mla_out = MLA_HEADS * MLA_V
    return {
        "x": nrm(ks[0], (BATCH, SEQ, D_MODEL), 1.0),
        "c": nrm(ks[1], (BATCH, D_MODEL), 1.0),
        "positions": jax.random.randint(ks[2], (BATCH, 1), 0, 65536, dtype=jnp.int32)
                     + jnp.arange(SEQ, dtype=jnp.int32)[None, :],
        "ada_w": nrm(ks[3], (DEPTH, D_MODEL, N_ADA * D_MODEL), 0.5 * D_MODEL ** -0.5),
        "ada_b": nrm(ks[4], (DEPTH, N_ADA * D_MODEL), 0.02),
        "norm1_g": 1.0 + nrm(ks[5], (DEPTH, D_MODEL), 0.02),
        "norm2_g": 1.0 + nrm(ks[6], (DEPTH, D_MODEL), 0.02),
        "w_in": nrm(ks[7], (DEPTH, D_MODEL, IN_COLS), D_MODEL ** -0.5),
        "mla_q_norm_g": 1.0 + nrm(ks[8], (DEPTH, MLA_Q_RANK), 0.02),
        "mla_kv_norm_g": 1.0 + nrm(ks[9], (DEPTH, MLA_KV_RANK), 0.02),
        "mla_w_uq": nrm(ks[10], (DEPTH, MLA_Q_RANK, MLA_HEADS * (MLA_NOPE + MLA_ROPE)), MLA_Q_RANK ** -0.5),
        "mla_w_ukv": nrm(ks[11], (DEPTH, MLA_KV_RANK, MLA_HEADS * (MLA_NOPE + MLA_V)), MLA_KV_RANK ** -0.5),
        "w_branch_mla": nrm(ks[12], (DEPTH, d_mla_out, D_MODEL), d_mla_out ** -0.5),
        "w_branch_sb": nrm(ks[13], (DEPTH, SB_WIDTH, D_MODEL), SB_WIDTH ** -0.5),
        "w_out": nrm(ks[14], (DEPTH, D_MODEL, D_MODEL), D_MODEL ** -0.5),
        "peer_w_q": nrm(ks[15], (DEPTH, D_MODEL, PEER_HEADS * PEER_DKEY), D_MODEL ** -0.5),
        "peer_sub_keys": nrm(ks[16], (DEPTH, PEER_HEADS, 2, PEER_NKEYS, PEER_DKEY // 2), (PEER_DKEY // 2) ** -0.5),
        "peer_u": nrm(ks[17], (DEPTH, PEER_N, D_MODEL), D_MODEL ** -0.5),
        "peer_v": nrm(ks[18], (DEPTH, PEER_N, D_MODEL), 1.0),
        "final_norm_g": 1.0 + nrm(ks[19], (D_MODEL,), 0.02),
    }


def reference(x, c, positions, ada_w, ada_b, norm1_g, norm2_g, w_in, mla_q_norm_g, mla_kv_norm_g,
              mla_w_uq, mla_w_ukv, w_branch_mla, w_branch_sb, w_out, peer_w_q, peer_sub_keys,
              peer_u, peer_v, final_norm_g):
    inv_freq = ROPE_THETA ** (-jnp.arange(0, MLA_ROPE, 2, dtype=jnp.float32) / MLA_ROPE)
    ang = positions.astype(jnp.float32)[..., None] * inv_freq
    cos = jnp.cos(ang).astype(x.dtype)
    sin = jnp.sin(ang).astype(x.dtype)
    for l in range(DEPTH):
        mod = jax.nn.silu(c) @ ada_w[l] + ada_b[l]
        sh1, sc1, gt1, sh2, sc2, gt2 = [m[:, None, :] for m in jnp.split(mod, N_ADA, axis=-1)]
        h = rmsnorm(x, norm1_g[l]) * (1.0 + sc1) + sh1
        x = x + gt1 * token_mixer(h, cos, sin, w_in[l], mla_q_norm_g[l], mla_kv_norm_g[l],
                                  mla_w_uq[l], mla_w_ukv[l], w_branch_mla[l], w_branch_sb[l], w_out[l])
        h = rmsnorm(x, norm2_g[l]) * (1.0 + sc2) + sh2
        x = x + gt2 * peer_ffn(h, peer_w_q[l], peer_sub_keys[l], peer_u[l], peer_v[l])
    return rmsnorm(x, final_norm_g)
```

```python
import numpy as np
import concourse.bass as bass
import concourse.mybir as mybir
from concourse.bass_utils import run_bass_kernel_spmd
from contextlib import ExitStack

F32 = mybir.dt.float32
BF16 = mybir.dt.bfloat16
I32 = mybir.dt.int32
U32 = mybir.dt.uint32
AF = mybir.ActivationFunctionType
ALU = mybir.AluOpType
AX = mybir.AxisListType

ENGS = ("pe", "act", "dve", "pool", "sp")


class Op:
    __slots__ = ("eng", "fn", "deps", "sig", "seq", "is_dma", "dsem", "dval", "prev", "extra")

    def __init__(self, eng, fn):
        self.eng = eng
        self.fn = fn
        self.deps = []
        self.sig = False
        self.seq = 0
        self.is_dma = False
        self.dsem = None
        self.dval = 0
        self.prev = None
        self.extra = None


class Prog:
    def __init__(self, nc, stack, n_dma_sems=12):
        self.nc = nc
        self.ops = {e: [] for e in ENGS}
        self.last_w = {}
        self.readers = {}
        self.stack = stack
        self.EP = 30000
        self.esem = {e: [] for e in ENGS}
        self.nsem = 0
        self.dsems = {}
        for q in ("sp", "pool", "act"):
            self.dsems[q] = [[self._newsem(), 0, None] for i in range(n_dma_sems)]
        self.dcur = {"sp": 0, "pool": 0, "act": 0}
        self.all_dma = []

    def _newsem(self):
        self.nsem += 1
        return self.stack.enter_context(self.nc.semaphore("sm%d" % self.nsem))

    @staticmethod
    def _k(b):
        if isinstance(b, tuple):
            return tuple(Prog._k(x) for x in b)
        if isinstance(b, (str, int)):
            return b
        return b.name

    def _deps(self, op, reads, writes):
        reads = [self._k(r) for r in reads]
        writes = [self._k(w) for w in writes]
        deps = []
        for r in reads:
            w = self.last_w.get(r)
            if w is not None:
                deps.append(w)
        for w_ in writes:
            w = self.last_w.get(w_)
            if w is not None:
                deps.append(w)
            deps.extend(self.readers.get(w_, ()))
        seen = set()
        for d in deps:
            if d is op or id(d) in seen:
                continue
            seen.add(id(d))
            if d.eng == "pe" and op.eng == "pe" and not d.is_dma:
                continue
            op.deps.append(d)
            if not d.is_dma:
                d.sig = True
        for r in reads:
            self.readers.setdefault(r, []).append(op)
        for w_ in writes:
            self.last_w[w_] = op
            self.readers[w_] = []

    def op(self, eng, fn, reads=(), writes=()):
        o = Op(eng, fn)
        self._deps(o, reads, writes)
        self.ops[eng].append(o)
        return o

    def dma(self, q, out, in_, reads=(), writes=(), **kw):
        o = Op(q, None)
        o.is_dma = True
        o.extra = (out, in_, kw)
        lst = self.dsems[q]
        i = self.dcur[q]
        self.dcur[q] = (i + 1) % len(lst)
        ent = lst[i]
        o.prev = ent[2]
        if ent[1] + 16 > 60000:
            ent[0] = self._newsem()
            ent[1] = 0
        ent[1] += 16
        ent[2] = o
        o.dsem = ent[0]
        o.dval = ent[1]
        self._deps(o, reads, writes)
        self.ops[q].append(o)
        self.all_dma.append(o)
        return o

    def barrier(self):
        lasts = []
        for e in ENGS:
            for o in reversed(self.ops[e]):
                if not o.is_dma and o.fn is not None:
                    o.sig = True
                    lasts.append(o)
                    break
        dm = []
        for q in self.dsems:
            for ent in self.dsems[q]:
                if ent[2] is not None:
                    dm.append(ent[2])
        for e in ENGS:
            o = Op(e, None)
            o.deps = [l for l in lasts] + dm
            self.ops[e].append(o)
        self.last_w = {}
        self.readers = {}

    def emit(self):
        nc = self.nc
        for e in ENGS:
            n = 0
            for o in self.ops[e]:
                if o.sig:
                    n += 1
                    o.seq = n
            while len(self.esem[e]) * self.EP < n + 1:
                self.esem[e].append(self._newsem())
        prog = self

        def run(e, eng):
            waited = {}
            dwaited = {}
            for o in prog.ops[e]:
                if o.is_dma and o.prev is not None:
                    k = id(o.prev.dsem)
                    if dwaited.get(k, 0) < o.prev.dval:
                        eng.wait_ge(o.prev.dsem, o.prev.dval)
                        dwaited[k] = o.prev.dval
                for d in o.deps:
                    if d.is_dma:
                        k = id(d.dsem)
                        if dwaited.get(k, 0) < d.dval:
                            eng.wait_ge(d.dsem, d.dval)
                            dwaited[k] = d.dval
                    else:
                        if waited.get(d.eng, 0) < d.seq:
                            eng.wait_ge(prog.esem[d.eng][(d.seq - 1) // prog.EP], (d.seq - 1) % prog.EP + 1)
                            waited[d.eng] = d.seq
                if o.is_dma:
                    out, in_, kw = o.extra
                    eng.dma_start(out=out, in_=in_, **kw).then_inc(o.dsem, 16)
                elif o.fn is not None:
                    ins = o.fn(eng)
                    if o.sig:
                        ins.then_inc(prog.esem[e][(o.seq - 1) // prog.EP], 1)

        with nc.Block() as block:
            @block.tensor
            def _(eng):
                run("pe", eng)

            @block.scalar
            def _(eng):
                run("act", eng)

            @block.vector
            def _(eng):
                run("dve", eng)

            @block.gpsimd
            def _(eng):
                run("pool", eng)

            @block.sync
            def _(eng):
                run("sp", eng)
import math

D = 2048
NKC = 16
SEQ = 4096
TOWN = 2048
EPS = 1e-6
NEXP = 16384


class Buf:
    def __init__(self, name, ap):
        self.name = name
        self.ap = ap

    def __getitem__(self, idx):
        return self.ap[idx]


class Arena:
    def __init__(self, big, nwords):
        self.big = big
        self.n = nwords
        self.off = 0
        self.uid = 0

    def mark(self):
        return self.off

    def reset(self, m):
        self.off = m

    def alloc(self, name, shape, dt=F32, parts=128):
        nel = 1
        for s in shape:
            nel *= s
        esz = 2 if dt == BF16 else 4
        nw = (nel * esz + 3) // 4
        nw = (nw + 15) // 16 * 16
        assert self.off + nw <= self.n, ("SBUF arena overflow", name, self.off, nw, self.n)
        v = self.big[0:parts, self.off:self.off + nw]
        self.off += nw
        if dt != F32:
            v = v.bitcast(dt)
        v = v[:, 0:nel]
        if len(shape) == 2:
            v = v.rearrange("p (a b) -> p a b", a=shape[0])
        elif len(shape) == 3:
            v = v.rearrange("p (a b c) -> p a b c", a=shape[0], b=shape[1])
        self.uid += 1
        return Buf("%s#%d" % (name, self.uid), v)


class Rot:
    def __init__(self, bufs):
        self.bufs = bufs
        self.i = 0

    def next(self):
        b = self.bufs[self.i % len(self.bufs)]
        self.i += 1
        return b


def build_program(upto=99, debug=False, dbg_names=()):
    nc = bass.Bass("TRN2", target_bir_lowering=False)

    def din(name, shape, dt=F32):
        return nc.dram_tensor(name, list(shape), dt, kind="ExternalInput").ap()

    def dscr(name, shape, dt=F32):
        kind = "ExternalOutput" if (debug and name in dbg_names) else "Internal"
        return nc.dram_tensor(name, list(shape), dt, kind=kind).ap()

    xT_nat = din("xT_nat", [D, SEQ])
    xT_own = din("xT_own", [D, TOWN])
    pos_nat = din("pos_nat", [1, SEQ], I32)
    pos_own = din("pos_own", [1, TOWN], I32)
    c_col = din("c_col", [128, 16])
    ada_w = din("ada_w", [D, 6 * D])
    ada_b_col = din("ada_b_col", [128, 96])
    g1_col = din("g1_col", [128, 16])
    g2_col = din("g2_col", [128, 16])
    gf_col = din("gf_col", [128, 16])
    w_kv = din("w_kv", [D, 2432])
    w_q1 = din("w_q1", [D, 5632])
    qn_g = din("qn_g", [128, 4])
    kvn_g = din("kvn_g", [128, 2])
    w_uq_n = din("w_uq_n", [512, 1024])
    w_uq_r = din("w_uq_r", [512, 512])
    w_uq_rs = din("w_uq_rs", [512, 512])
    w_ukv_k = din("w_ukv_k", [256, 1024])
    w_ukv_v = din("w_ukv_v", [256, 1024])
    w_bm = din("w_bm", [1024, D])
    w_bs = din("w_bs", [1024, D])
    w_out = din("w_out", [D, D])
    w_pq = din("w_pq", [D, D])
    k1T = din("k1T", [128, 8, 128])
    k2T = din("k2T", [128, 8, 128])
    big_tabs = upto >= 9
    uT = din("uT", [D, NEXP] if big_tabs else [128, 128])
    v_tab = din("v_tab", [NEXP, D] if big_tabs else [128, 128])
    invf2 = din("invf2", [64, 1])
    sgn = din("sgn", [64, 1])
    mask_mla = din("mask_mla", [128, 8, 512])
    mask_sb = din("mask_sb", [128, 8, 512])
    ident_d = din("ident", [128, 128])
    tmat_d = din("tmat", [128, 128])
    iota_d = din("iota", [128, 128])
    umat_d = din("umat", [128, 128])

    outT = nc.dram_tensor("outT", [D, TOWN], F32, kind="ExternalOutput").ap()

    ckvT = dscr("ckvT", [256, SEQ])
    krT = dscr("krT", [64, SEQ])
    krsT = dscr("krsT", [64, SEQ])
    sbkT = dscr("sbkT", [1024, SEQ], BF16)
    sbv = dscr("sbv", [SEQ, 1024], BF16)
    cqT = dscr("cqT", [512, TOWN])
    sbqT = dscr("sbqT", [1024, TOWN], BF16)
    gaT = dscr("gaT", [D, TOWN])
    gbT = dscr("gbT", [D, TOWN])
    KnT = dscr("KnT", [8, 128, SEQ], BF16)
    vmla = dscr("vmla", [SEQ, 1024], BF16)
    kropeT = dscr("kropeT", [64, SEQ], BF16)
    QnT = dscr("QnT", [8, 128, TOWN], BF16)
    QrT = dscr("QrT", [8, 64, TOWN], BF16)
    x1T = dscr("x1T", [D, TOWN])
    pqT = dscr("pqT", [D, TOWN])
    WT = dscr("WT", [128, 128, TOWN], BF16)
    AT = dscr("AT", [128, 128, TOWN], BF16)
    h2s = dscr("h2s", [128, 16, TOWN], BF16)
    dbg_mod = dscr("dbg_mod", [128, 96])
    dbg_attn = dscr("dbg_attn", [128, 16, TOWN], BF16)
    dbg_h2 = dscr("dbg_h2", [128, 16, TOWN], BF16)
    dbg_peer = dscr("dbg_peer", [128, 16, TOWN])

    with ExitStack() as st:
        P = Prog(nc, st, n_dma_sems=12)
        NW = 207 * 256
        big = st.enter_context(nc.sbuf_tensor("big", [128, NW], F32))
        psb = [Buf("ps%d" % i, st.enter_context(nc.psum_tensor("ps%d" % i, [128, 512], F32))[:]) for i in range(8)]
        A = Arena(big, NW)

        def mm(ps_ap, lhsT, rhs, start, stop, reads, writes, skip=False):
            if skip:
                return P.op("pe", lambda e: e.matmul(ps_ap, lhsT=lhsT, rhs=rhs, start=start, stop=stop, skip_group_check=True), reads=reads, writes=writes)
            return P.op("pe", lambda e: e.matmul(ps_ap, lhsT=lhsT, rhs=rhs, start=start, stop=stop), reads=reads, writes=writes)

        def act(out, in_, func, reads, writes, **kw):
            return P.op("act", lambda e: e.activation(out=out, in_=in_, func=func, **kw), reads=reads, writes=writes)

        def tt(eng, out, in0, in1, op, reads, writes):
            return P.op(eng, lambda e: e.tensor_tensor(out=out, in0=in0, in1=in1, op=op), reads=reads, writes=writes)

        def ts(eng, out, in0, s1, s2, op0, op1, reads, writes):
            if op1 is None:
                return P.op(eng, lambda e: e.tensor_scalar(out=out, in0=in0, scalar1=s1, scalar2=None, op0=op0), reads=reads, writes=writes)
            return P.op(eng, lambda e: e.tensor_scalar(out=out, in0=in0, scalar1=s1, scalar2=s2, op0=op0, op1=op1), reads=reads, writes=writes)

        def stt(out, in0, scalar, in1, op0, op1, reads, writes):
            return P.op("dve", lambda e: e.scalar_tensor_tensor(out=out, in0=in0, scalar=scalar, in1=in1, op0=op0, op1=op1), reads=reads, writes=writes)

        def cp(eng, out, in_, reads, writes):
            if eng == "act":
                return P.op("act", lambda e: e.copy(out=out, in_=in_), reads=reads, writes=writes)
            return P.op(eng, lambda e: e.tensor_copy(out=out, in_=in_), reads=reads, writes=writes)

        cast_i = [0]

        def cast(out, in_, reads, writes):
            cast_i[0] += 1
            return cp("dve" if cast_i[0] % 2 else "act", out, in_, reads, writes)

        ident_f = A.alloc("ident_f", [128])
        ident_b = A.alloc("ident_b", [128], BF16)
        ones_f = A.alloc("ones_f", [128])
        ones_b = A.alloc("ones_b", [128], BF16)
        tmat_f = A.alloc("tmat_f", [128])
        mod = A.alloc("mod", [96])
        A1 = A.alloc("A1", [16]); B1 = A.alloc("B1", [16]); A2 = A.alloc("A2", [16]); B2 = A.alloc("B2", [16])
        GT1 = A.alloc("GT1", [16]); GT2 = A.alloc("GT2", [16]); GF = A.alloc("GF", [16])
        g1s = A.alloc("g1s", [16]); g2s = A.alloc("g2s", [16])
        qng = A.alloc("qng", [4]); kvng = A.alloc("kvng", [2])
        invf = A.alloc("invf", [1], parts=64); sgnc = A.alloc("sgnc", [1], parts=64)
        PERS = ["pers"]

        P.dma("sp", ident_f.ap, ident_d, writes=[ident_f])
        P.dma("sp", tmat_f.ap, tmat_d, writes=[tmat_f])
        P.dma("sp", g1s.ap, g1_col, writes=[g1s])
        P.dma("sp", g2s.ap, g2_col, writes=[g2s])
        P.dma("sp", GF.ap, gf_col, writes=[GF])
        P.dma("sp", qng.ap, qn_g, writes=[qng])
        P.dma("sp", kvng.ap, kvn_g, writes=[kvng])
        P.dma("sp", invf.ap, invf2, writes=[invf])
        P.dma("sp", sgnc.ap, sgn, writes=[sgnc])
        cp("dve", ident_b.ap, ident_f.ap, [ident_f], [ident_b])
        P.op("dve", lambda e: e.memset(ones_f.ap, 1.0), writes=[ones_f])
        P.op("dve", lambda e: e.memset(ones_b.ap, 1.0), writes=[ones_b])
        m0 = A.mark()

        ccol = A.alloc("ccol", [16]); scol = A.alloc("scol", [16]); adab = A.alloc("adab", [96])
        P.dma("sp", ccol.ap, c_col, writes=[ccol])
        P.dma("sp", adab.ap, ada_b_col, writes=[adab])
        act(scol.ap, ccol.ap, AF.Silu, [ccol], [scol])
        scb = A.alloc("scb", [16], BF16)
        cp("dve", scb.ap, scol.ap, [scol], [scb])
        wst = Rot([A.alloc("adaw", [6 * D]) for _ in range(2)])
        wbb = Rot([A.alloc("adawb", [6 * D], BF16) for _ in range(2)])
        for kc in range(16):
            wj = wst.next(); wb_ = wbb.next()
            for q4 in range(4):
                cs = slice(q4 * 3072, (q4 + 1) * 3072)
                P.dma("sp", wj[:, cs], ada_w[kc * 128:(kc + 1) * 128, cs], writes=[(wj, q4)])
                cast(wb_[:, cs], wj[:, cs], [(wj, q4)], [(wb_, q4)])
            for j in range(96):
                P.op("pe", lambda e, o=psb[0][:, j:j + 1], l=wb_[:, j * 128:(j + 1) * 128], r=scb[:, kc:kc + 1], st=(kc == 0 and j == 0), sp=(kc == 15):
                     e.matmul(o, lhsT=l, rhs=r, start=st, stop=sp, skip_group_check=True), reads=[(wb_, j // 24), scb], writes=[psb[0]])
        tt("dve", mod.ap, psb[0][:, 0:96], adab.ap, ALU.add, [psb[0], adab], [mod])
        tmpc = A.alloc("tmpc", [16])
        ts("dve", tmpc.ap, mod[:, 16:32], 1.0, None, ALU.add, None, [mod], [tmpc])
        tt("dve", A1.ap, tmpc.ap, g1s.ap, ALU.mult, [tmpc, g1s], [A1])
        cp("dve", B1.ap, mod[:, 0:16], [mod], [B1])
        cp("dve", GT1.ap, mod[:, 32:48], [mod], [GT1])
        tmpc2 = A.alloc("tmpc2", [16])
        ts("dve", tmpc2.ap, mod[:, 64:80], 1.0, None, ALU.add, None, [mod], [tmpc2])
        tt("dve", A2.ap, tmpc2.ap, g2s.ap, ALU.mult, [tmpc2, g2s], [A2])
        cp("dve", B2.ap, mod[:, 48:64], [mod], [B2])
        cp("dve", GT2.ap, mod[:, 80:96], [mod], [GT2])
        if debug:
            P.dma("pool", dbg_mod, mod.ap, reads=[mod], writes=["dbg_mod"])
        P.barrier()
        A.reset(m0)

        def norm_tokens(src, t0, T, Acol, Bcol, ncs, dst_buf=None, dst_dram=None, inv_n=1.0 / D):
            srcv = src.rearrange("(kc p) t -> p kc t", p=128)
            nmk = A.mark()
            xbs = Rot([A.alloc("nx", [ncs, 512]) for _ in range(2)])
            sqs = Rot([A.alloc("nsq", [512]) for _ in range(2)])
            tms = Rot([A.alloc("ntm", [512]) for _ in range(3)])
            r1 = A.alloc("nr1", [512]); rstd = A.alloc("nrstd", [512])
            ssq = psb[7]
            for tg in range(T // 512):
                xb = xbs.next()
                P.dma("sp", xb.ap, srcv[:, :, t0 + tg * 512: t0 + (tg + 1) * 512], writes=[xb])
                for kc in range(ncs):
                    sq = sqs.next()
                    act(sq.ap, xb[:, kc, :], AF.Square, [xb], [sq])
                    mm(ssq.ap, ones_f.ap, sq.ap, kc == 0, kc == ncs - 1, [sq, ones_f], [ssq])
                act(r1.ap, ssq.ap, AF.Sqrt, [ssq], [r1], scale=inv_n, bias=EPS)
                P.op("dve", lambda e, o=rstd.ap, i=r1.ap: e.reciprocal(out=o, in_=i), reads=[r1], writes=[rstd])
                for kc in range(ncs):
                    tm = tms.next()
                    tt("dve", tm.ap, xb[:, kc, :], rstd.ap, ALU.mult, [xb, rstd], [tm])
                    if dst_buf is not None:
                        o = dst_buf[:, kc, tg * 512:(tg + 1) * 512]
                        kw = dict(scale=Acol[:, kc:kc + 1])
                        if Bcol is not None:
                            kw["bias"] = Bcol[:, kc:kc + 1]
                        act(o, tm.ap, AF.Identity, [tm, "pers"], [(dst_buf, kc, tg)], **kw)
                    else:
                        act(tm.ap, tm.ap, AF.Identity, [tm, "pers"], [tm], scale=Acol[:, kc:kc + 1])
                        P.dma("pool", dst_dram[kc * 128:(kc + 1) * 128, tg * 512:(tg + 1) * 512], tm.ap, reads=[tm], writes=["ndst"])
            P.barrier()
            A.reset(nmk)

        def load_w(wd, r0, c0, ncols, dst, nkc, stg):
            for kc in range(nkc):
                s = stg.next()
                P.dma("sp", s[:, 0:ncols], wd[r0 + kc * 128: r0 + (kc + 1) * 128, c0:c0 + ncols], writes=[s])
                cast(dst[:, kc, 0:ncols], s[:, 0:ncols], [s], [(dst, kc)])

        def load_wblk(wd, c0, dst, nkc, stg):
            s_ = stg.next()
            P.dma("sp", s_[:, 0:nkc, :], wd.rearrange("(kc p) n -> p kc n", p=128)[:, 0:nkc, c0:c0 + 128], writes=[s_])
            cast(dst.ap, s_[:, 0:nkc, :], [s_], [(dst, kc) for kc in range(nkc)])

        def wreads(dst, nkc):
            return [(dst, kc) for kc in range(nkc)]

        psrot = Rot([psb[0], psb[1], psb[2], psb[3]])

        def kv_phase():
            for half in range(2):
                mk = A.mark()
                hT = A.alloc("hT", [16, 2048], BF16)
                norm_tokens(xT_nat, half * 2048, 2048, A1, B1, 16, dst_buf=hT)
                hreads = [(hT, tg) for tg in range(4)]
                wstg = Rot([A.alloc("wstg", [1024]) for _ in range(3)])
                wbr = Rot([A.alloc("wb", [16, 1024], BF16) for _ in range(2)])
                ost = Rot([A.alloc("ost", [512]) for _ in range(4)])
                wb = wbr.next()
                load_w(w_kv, 0, 0, 384, wb, 16, wstg)
                for tg in range(4):
                    tcols = slice(tg * 512, (tg + 1) * 512)
                    gcols = slice(half * 2048 + tg * 512, half * 2048 + (tg + 1) * 512)
                    for blk in range(4):
                        ps = psrot.next()
                        if blk < 2:
                            c_lo, m = blk * 128, 128
                        else:
                            c_lo, m = 256 + (blk - 2) * 64, 64
                        for kc in range(16):
                            mm(ps[0:m, :], wb[:, kc, c_lo:c_lo + m], hT[:, kc, tcols], kc == 0, kc == 15, [(wb, kc), (hT, kc, tg)], [ps])
                        o = ost.next()
                        cast(o[0:m, :], ps[0:m, :], [ps], [o])
                        if blk < 2:
                            P.dma("pool", ckvT[blk * 128:(blk + 1) * 128, gcols], o.ap, reads=[o], writes=["ckvT"])
                        elif blk == 2:
                            P.dma("pool", krT[:, gcols], o[0:64, :], reads=[o], writes=["krT"])
                        else:
                            P.dma("pool", krsT[:, gcols], o[0:64, :], reads=[o], writes=["krsT"])
                obs = Rot([A.alloc("obs", [512], BF16) for _ in range(4)])
                wb = wbr.next()
                load_w(w_kv, 0, 384, 1024, wb, 16, wstg)
                for tg in range(4):
                    tcols = slice(tg * 512, (tg + 1) * 512)
                    gcols = slice(half * 2048 + tg * 512, half * 2048 + (tg + 1) * 512)
                    for blk in range(8):
                        ps = psrot.next()
                        for kc in range(16):
                            mm(ps.ap, wb[:, kc, blk * 128:(blk + 1) * 128], hT[:, kc, tcols], kc == 0, kc == 15, [(wb, kc), (hT, kc, tg)], [ps])
                        o = obs.next()
                        cast(o.ap, ps.ap, [ps], [o])
                        P.dma("pool", sbkT[blk * 128:(blk + 1) * 128, gcols], o.ap, reads=[o], writes=["sbkT"])
                wb = wbr.next()
                load_w(w_kv, 0, 1408, 1024, wb, 16, wstg)
                for tt_ in range(16):
                    tg = tt_ // 4
                    for cb in range(2):
                        ps = psrot.next()
                        for kc in range(16):
                            mm(ps.ap, hT[:, kc, tt_ * 128:(tt_ + 1) * 128], wb[:, kc, cb * 512:(cb + 1) * 512], kc == 0, kc == 15, [(wb, kc), (hT, kc, tg)], [ps])
                        o = obs.next()
                        cast(o.ap, ps.ap, [ps], [o])
                        r0 = half * 2048 + tt_ * 128
                        P.dma("pool", sbv[r0:r0 + 128, cb * 512:(cb + 1) * 512], o.ap, reads=[o], writes=["sbv"])
                P.barrier()
                A.reset(mk)

        if upto >= 1:
            kv_phase()

        def q_phase():
            mk = A.mark()
            hT = A.alloc("hT", [16, 2048], BF16)
            norm_tokens(xT_own, 0, 2048, A1, B1, 16, dst_buf=hT)
            wstg = Rot([A.alloc("wstg", [1024]) for _ in range(3)])
            wbr = Rot([A.alloc("wb", [16, 1024], BF16) for _ in range(2)])
            ost = Rot([A.alloc("ost", [512]) for _ in range(4)])
            obs = Rot([A.alloc("obs", [512], BF16) for _ in range(4)])
            groups = [(0, 512, "f32", cqT, 0), (512, 1024, "bf16", sbqT, 0),
                      (1536, 1024, "sig", gaT, 0), (2560, 1024, "sig", gaT, 1024),
                      (3584, 1024, "sig", gbT, 0), (4608, 1024, "sig", gbT, 1024)]
            for (c0, ncols, kind, dst, roff) in groups:
                wb = wbr.next()
                load_w(w_q1, 0, c0, ncols, wb, 16, wstg)
                for tg in range(4):
                    tcols = slice(tg * 512, (tg + 1) * 512)
                    for blk in range(ncols // 128):
                        ps = psrot.next()
                        for kc in range(16):
                            mm(ps.ap, wb[:, kc, blk * 128:(blk + 1) * 128], hT[:, kc, tcols], kc == 0, kc == 15, [(wb, kc), (hT, kc, tg)], [ps])
                        rows = slice(roff + blk * 128, roff + (blk + 1) * 128)
                        if kind == "bf16":
                            o = obs.next()
                            cast(o.ap, ps.ap, [ps], [o])
                        elif kind == "f32":
                            o = ost.next()
                            cast(o.ap, ps.ap, [ps], [o])
                        else:
                            o = ost.next()
                            act(o.ap, ps.ap, AF.Sigmoid, [ps], [o])
                        P.dma("pool", dst[rows, tcols], o.ap, reads=[o], writes=[("qd", id(dst))])
            P.barrier()
            A.reset(mk)

        if upto >= 2:
            q_phase()

        TWO_PI = 2.0 * math.pi
        C1 = 6.28125
        C2 = float(np.float32(TWO_PI - C1))
        MAGIC = 12582912.0

        def rope_tables(pos_d, t0, n, cos2, sin2, tmp):
            pi_, ang, kf, r = tmp[0], tmp[1], tmp[2], tmp[3]
            pint = tmp[4]
            P.dma("sp", pint[0:64, 0:n], pos_d[0:1, t0:t0 + n].broadcast_to([64, n]), writes=[pint])
            cp("dve", ang[0:64, 0:n], pint[0:64, 0:n], [pint], [ang])
            ts("dve", ang[0:64, 0:n], ang[0:64, 0:n], invf[0:64, 0:1], None, ALU.mult, None, [ang, "pers"], [ang])
            ts("dve", kf[0:64, 0:n], ang[0:64, 0:n], 1.0 / TWO_PI, MAGIC, ALU.mult, ALU.add, [ang], [kf])
            ts("dve", kf[0:64, 0:n], kf[0:64, 0:n], MAGIC, None, ALU.subtract, None, [kf], [kf])
            stt(r[0:64, 0:n], kf[0:64, 0:n], -C1, ang[0:64, 0:n], ALU.mult, ALU.add, [kf, ang], [r])
            stt(r[0:64, 0:n], kf[0:64, 0:n], -C2, r[0:64, 0:n], ALU.mult, ALU.add, [kf, r], [r])
            ts("dve", kf[0:64, 0:n], r[0:64, 0:n], -math.pi, TWO_PI, ALU.is_lt, ALU.mult, [r], [kf])
            tt("dve", r[0:64, 0:n], r[0:64, 0:n], kf[0:64, 0:n], ALU.add, [r, kf], [r])
            ts("dve", kf[0:64, 0:n], r[0:64, 0:n], math.pi, -TWO_PI, ALU.is_gt, ALU.mult, [r], [kf])
            tt("dve", r[0:64, 0:n], r[0:64, 0:n], kf[0:64, 0:n], ALU.add, [r, kf], [r])
            ts("dve", r[0:64, 0:n], r[0:64, 0:n], math.pi, -math.pi, ALU.min, ALU.max, [r], [r])
            act(pi_[0:64, 0:n], r[0:64, 0:n], AF.Sin, [r], [pi_])
            ts("dve", sin2, pi_[0:64, 0:n], sgnc[0:64, 0:1], None, ALU.mult, None, [pi_, "pers"], ["sin2"])
            ts("dve", kf[0:64, 0:n], r[0:64, 0:n], math.pi / 2, -TWO_PI, ALU.is_gt, ALU.mult, [r], [kf])
            tt("dve", kf[0:64, 0:n], kf[0:64, 0:n], r[0:64, 0:n], ALU.add, [kf, r], [kf])
            ts("dve", kf[0:64, 0:n], kf[0:64, 0:n], math.pi / 2, math.pi, ALU.add, ALU.min, [kf], [kf])
            act(cos2, kf[0:64, 0:n], AF.Sin, [kf], ["cos2"])

        def small_norm(src, T, ncs, gcol, dst, inv_n):
            norm_tokens(src, 0, T, gcol, None, ncs, dst_buf=dst, inv_n=inv_n)

        def mla_prep():
            mk = A.mark()
            kvnT = A.alloc("kvnT", [2, SEQ], BF16)
            small_norm(ckvT, SEQ, 2, kvng, kvnT, 1.0 / 256)
            qnT = A.alloc("qnT", [4, TOWN], BF16)
            small_norm(cqT, TOWN, 4, qng, qnT, 1.0 / 512)
            wstg = Rot([A.alloc("wstg", [1024]) for _ in range(3)])
            wk = A.alloc("wk", [2, 1024], BF16); wv = A.alloc("wv", [2, 1024], BF16)
            wn = A.alloc("wn", [4, 1024], BF16); wr = A.alloc("wr", [4, 512], BF16); wrs = A.alloc("wrs", [4, 512], BF16)
            load_w(w_ukv_k, 0, 0, 1024, wk, 2, wstg)
            load_w(w_ukv_v, 0, 0, 1024, wv, 2, wstg)
            load_w(w_uq_n, 0, 0, 1024, wn, 4, wstg)
            load_w(w_uq_r, 0, 0, 512, wr, 4, wstg)
            load_w(w_uq_rs, 0, 0, 512, wrs, 4, wstg)
            obs = Rot([A.alloc("obs", [512], BF16) for _ in range(4)])
            kvr = [(kvnT, tg) for tg in range(8)]
            for h in range(8):
                for tg in range(8):
                    ps = psrot.next()
                    for c in range(2):
                        mm(ps.ap, wk[:, c, h * 128:(h + 1) * 128], kvnT[:, c, tg * 512:(tg + 1) * 512], c == 0, c == 1, [(wk, c), (kvnT, c, tg)], [ps])
                    o = obs.next()
                    cast(o.ap, ps.ap, [ps], [o])
                    P.dma("pool", KnT[h, :, tg * 512:(tg + 1) * 512], o.ap, reads=[o], writes=["KnT"])
            for tt_ in range(32):
                for cb in range(2):
                    ps = psrot.next()
                    for c in range(2):
                        mm(ps.ap, kvnT[:, c, tt_ * 128:(tt_ + 1) * 128], wv[:, c, cb * 512:(cb + 1) * 512], c == 0, c == 1, [(wv, c), (kvnT, c, tt_ // 4)], [ps])
                    o = obs.next()
                    cast(o.ap, ps.ap, [ps], [o])
                    P.dma("pool", vmla[tt_ * 128:(tt_ + 1) * 128, cb * 512:(cb + 1) * 512], o.ap, reads=[o], writes=["vmla"])
            for h in range(8):
                for tg in range(4):
                    ps = psrot.next()
                    for c in range(4):
                        mm(ps.ap, wn[:, c, h * 128:(h + 1) * 128], qnT[:, c, tg * 512:(tg + 1) * 512], c == 0, c == 3, [(wn, c), (qnT, c, tg)], [ps])
                    o = obs.next()
                    cast(o.ap, ps.ap, [ps], [o])
                    P.dma("pool", QnT[h, :, tg * 512:(tg + 1) * 512], o.ap, reads=[o], writes=["QnT"])
            tmp = [A.alloc("rt%d" % i, [512]) for i in range(4)] + [Buf("pint", A.alloc("pint", [512]).ap.bitcast(I32))]
            cos2 = A.alloc("cos2", [512]); sin2 = A.alloc("sin2", [512])
            kx = Rot([A.alloc("kx", [512]) for _ in range(2)]); ky = Rot([A.alloc("ky", [512]) for _ in range(2)])
            m1 = A.alloc("m1", [512]); m2 = A.alloc("m2", [512])
            for tg in range(8):
                cols = slice(tg * 512, (tg + 1) * 512)
                rope_tables(pos_nat, tg * 512, 512, cos2[0:64, :], sin2[0:64, :], tmp)
                a = kx.next(); b = ky.next()
                P.dma("sp", a[0:64, :], krT[:, cols], writes=[a])
                P.dma("sp", b[0:64, :], krsT[:, cols], writes=[b])
                tt("dve", m1[0:64, :], a[0:64, :], cos2[0:64, :], ALU.mult, [a, "cos2"], [m1])
                tt("dve", m2[0:64, :], b[0:64, :], sin2[0:64, :], ALU.mult, [b, "sin2"], [m2])
                o = obs.next()
                tt("dve", o[0:64, :], m1[0:64, :], m2[0:64, :], ALU.add, [m1, m2], [o])
                P.dma("pool", kropeT[:, cols], o[0:64, :], reads=[o], writes=["kropeT"])
            for tg in range(4):
                cols = slice(tg * 512, (tg + 1) * 512)
                rope_tables(pos_own, tg * 512, 512, cos2[0:64, :], sin2[0:64, :], tmp)
                for h in range(8):
                    psa = psrot.next(); psc = psrot.next()
                    for c in range(4):
                        mm(psa[0:64, :], wr[:, c, h * 64:(h + 1) * 64], qnT[:, c, cols], c == 0, c == 3, [(wr, c), (qnT, c, tg)], [psa])
                    for c in range(4):
                        mm(psc[0:64, :], wrs[:, c, h * 64:(h + 1) * 64], qnT[:, c, cols], c == 0, c == 3, [(wrs, c), (qnT, c, tg)], [psc])
                    tt("dve", m1[0:64, :], psa[0:64, :], cos2[0:64, :], ALU.mult, [psa, "cos2"], [m1])
                    tt("dve", m2[0:64, :], psc[0:64, :], sin2[0:64, :], ALU.mult, [psc, "sin2"], [m2])
                    o = obs.next()
                    tt("dve", o[0:64, :], m1[0:64, :], m2[0:64, :], ALU.add, [m1, m2], [o])
                    P.dma("pool", QrT[h, :, cols], o[0:64, :], reads=[o], writes=["QrT"])
            P.barrier()
            A.reset(mk)

        if upto >= 3:
            mla_prep()

        attn_mark = A.mark()
        OT = A.alloc("OT", [8, TOWN], BF16)
        SBT = A.alloc("SBT", [8, TOWN], BF16)

        def mla_attn():
            mk = A.mark()
            scale = 192.0 ** -0.5
            mkf = A.alloc("mkf", [8, 512]); mkb = A.alloc("mkb", [8, 512], BF16)
            P.dma("sp", mkf.ap, mask_mla, writes=[mkf])
            cp("dve", mkb.ap, mkf.ap, [mkf], [mkb])
            kro = A.alloc("kro", [SEQ], BF16)
            P.dma("sp", kro[0:64, :], kropeT, writes=[kro])
            kn = Rot([A.alloc("kn", [SEQ], BF16) for _ in range(2)])
            vh = Rot([A.alloc("vh", [32, 128], BF16) for _ in range(2)])
            qn = Rot([A.alloc("qn", [TOWN], BF16) for _ in range(2)])
            qr = Rot([A.alloc("qr", [TOWN], BF16) for _ in range(2)])
            pts = Rot([A.alloc("pt", [512], BF16) for _ in range(3)])
            rden = A.alloc("rden", [512])
            sps = Rot([psb[0], psb[1], psb[2]])
            acc = psb[4]; den = psb[5]
            hb = {}

            def load_head(h):
                k_ = kn.next(); v_ = vh.next(); q_ = qn.next(); r_ = qr.next()
                P.dma("sp", k_.ap, KnT[h], writes=[k_])
                P.dma("sp", v_.ap, vmla[:, h * 128:(h + 1) * 128].rearrange("(kb p) d -> p kb d", p=128), writes=[v_])
                P.dma("sp", q_.ap, QnT[h], writes=[q_])
                P.dma("sp", r_[0:64, :], QrT[h], writes=[r_])
                hb[h] = (k_, v_, q_, r_)

            units = [(h, g, kb) for h in range(8) for g in range(4) for kb in range(8 * g + 8)]
            sbank = {}

            def stage1(n):
                h, g, kb = units[n]
                k_, v_, q_, r_ = hb[h]
                qc = slice(g * 512, (g + 1) * 512); kc_ = slice(kb * 128, (kb + 1) * 128)
                s_ = sps.next()
                sbank[n] = s_
                mm(s_.ap, k_[:, kc_], q_[:, qc], True, False, [k_, q_], [s_])
                mm(s_.ap, kro[0:64, kc_], r_[0:64, qc], False, True, [kro, r_], [s_])

            def stage2(n):
                h, g, kb = units[n]
                k_, v_, q_, r_ = hb[h]
                nkb = 8 * g + 8
                qc = slice(g * 512, (g + 1) * 512)
                s_ = sbank.pop(n)
                pt = pts.next()
                act(pt.ap, s_.ap, AF.Exp, [s_], [pt], scale=scale)
                j = kb - 8 * g
                if j >= 0:
                    tt("dve", pt.ap, pt.ap, mkb[:, j, :], ALU.mult, [pt, mkb], [pt])
                mm(acc.ap, v_[:, kb, :], pt.ap, kb == 0, kb == nkb - 1, [v_, pt], [acc])
                mm(den.ap, ones_b.ap, pt.ap, kb == 0, kb == nkb - 1, [ones_b, pt], [den])
                if kb == nkb - 1:
                    P.op("dve", lambda e, o=rden.ap, i=den.ap: e.reciprocal(out=o, in_=i), reads=[den], writes=[rden])
                    tt("dve", OT[:, h, qc], acc.ap, rden.ap, ALU.mult, [acc, rden], [(OT, h, g)])

            load_head(0)
            load_head(1)
            N = len(units)
            for n in range(N + 1):
                if n < N:
                    stage1(n)
                if n >= 1:
                    stage2(n - 1)
                    hh, gg, kk = units[n - 1]
                    if gg == 3 and kk == 31 and hh + 2 < 8:
                        load_head(hh + 2)
            P.barrier()
            A.reset(mk)

        def sb_attn():
            mk = A.mark()
            scale = 128.0 ** -0.5
            mkf = A.alloc("mkf", [8, 512]); mkb = A.alloc("mkb", [8, 512], BF16)
            P.dma("sp", mkf.ap, mask_sb, writes=[mkf])
            cp("dve", mkb.ap, mkf.ap, [mkf], [mkb])
            umf = A.alloc("umf", [128]); tmf = A.alloc("tmf", [128]); tmb = A.alloc("tmb", [128], BF16); umb = A.alloc("umb", [128], BF16)
            P.dma("sp", umf.ap, umat_d, writes=[umf])
            P.dma("sp", tmf.ap, tmat_d, writes=[tmf])
            cp("dve", umb.ap, umf.ap, [umf], [umb])
            cp("dve", tmb.ap, tmf.ap, [tmf], [tmb])
            kn = Rot([A.alloc("kn", [SEQ], BF16) for _ in range(4)])
            vh = Rot([A.alloc("vh", [32, 128], BF16) for _ in range(4)])
            qn = Rot([A.alloc("qn", [TOWN], BF16) for _ in range(4)])
            es = Rot([A.alloc("es", [512]) for _ in range(4)])
            ens = Rot([A.alloc("en", [512]) for _ in range(3)])
            sp_ = Rot([A.alloc("sp", [512]) for _ in range(3)])
            his = Rot([A.alloc("hi", [512], BF16) for _ in range(5)])
            los = Rot([A.alloc("lo", [512], BF16) for _ in range(5)])
            wts = Rot([A.alloc("wt", [512], BF16) for _ in range(3)])
            zps = Rot([psb[0], psb[1], psb[2], psb[3]])
            Lb = [psb[4], psb[5]]
            accs = [psb[6], psb[7]]
            hb = {}

            def load_head(h):
                k_ = kn.next(); v_ = vh.next(); q_ = qn.next()
                P.dma("sp", k_.ap, sbkT[h * 128:(h + 1) * 128, :], writes=[k_])
                P.dma("sp", v_.ap, sbv[:, h * 128:(h + 1) * 128].rearrange("(kb p) d -> p kb d", p=128), writes=[v_])
                P.dma("sp", q_.ap, sbqT[h * 128:(h + 1) * 128, :], writes=[q_])
                hb[h] = (k_, v_, q_)

            units = []
            for hp in range(4):
                for g in range(4):
                    for kb in range(8 * g + 7, -1, -1):
                        units.append((2 * hp, g, kb))
                        units.append((2 * hp + 1, g, kb))
            st_ = {}

            def stageA(n):
                h, g, kb = units[n]
                k_, v_, q_ = hb[h]
                qc = slice(g * 512, (g + 1) * 512); kc_ = slice(kb * 128, (kb + 1) * 128)
                z = zps.next()
                mm(z.ap, k_[:, kc_], q_[:, qc], True, True, [k_, q_], [z])
                st_[n] = [z]

            def stageB(n):
                h, g, kb = units[n]
                z = st_[n][0]
                e_ = es.next()
                act(e_.ap, z.ap, AF.Exp, [z], [e_], scale=scale)
                s_ = sp_.next()
                act(s_.ap, e_.ap, AF.Ln, [e_], [s_], bias=1.0, scale=1.0)
                j = kb - 8 * g
                if j >= 0:
                    tt("dve", s_.ap, s_.ap, mkf[:, j, :], ALU.mult, [s_, mkf], [s_])
                hi = his.next(); lo = los.next()
                cp("dve", hi.ap, s_.ap, [s_], [hi])
                tt("pool", lo.ap, s_.ap, hi.ap, ALU.subtract, [s_, hi], [lo])
                st_[n] = [z, hi, lo, None, e_]

            def stageB2(n):
                h, g, kb = units[n]
                nkb = 8 * g + 8
                first = (kb == nkb - 1)
                L = Lb[n % 2]
                z, hi, lo, _, e_ = st_[n]
                if not first:
                    phi, plo = st_[n - 2][1], st_[n - 2][2]
                    mm(L.ap, umb.ap, phi.ap, False, False, [umb, phi], [L], skip=True)
                    mm(L.ap, umb.ap, plo.ap, False, False, [umb, plo], [L], skip=True)
                mm(L.ap, tmb.ap, hi.ap, first, False, [tmb, hi], [L], skip=True)
                mm(L.ap, tmb.ap, lo.ap, False, kb == 0, [tmb, lo], [L], skip=True)
                en = ens.next()
                act(en.ap, L.ap, AF.Exp, [L], [en], scale=-1.0)
                st_[n][3] = en

            def stageC(n):
                h, g, kb = units[n]
                k_, v_, q_ = hb[h]
                nkb = 8 * g + 8
                first = (kb == nkb - 1)
                qc = slice(g * 512, (g + 1) * 512)
                acc = accs[n % 2]
                en = st_[n][3]; e_ = st_[n][4]
                w_ = wts.next()
                tt("dve", w_.ap, e_.ap, en.ap, ALU.mult, [e_, en], [w_])
                j = kb - 8 * g
                if j >= 0:
                    tt("dve", w_.ap, w_.ap, mkb[:, j, :], ALU.mult, [w_, mkb], [w_])
                mm(acc.ap, v_[:, kb, :], w_.ap, first, kb == 0, [v_, w_], [acc])
                if kb == 0:
                    cp("act", SBT[:, h, qc], acc.ap, [acc], [(SBT, h, g)])
                if n >= 4:
                    st_.pop(n - 4, None)

            for h in range(4):
                load_head(h)
            N = len(units)
            for i in range(N + 3):
                if i < N:
                    stageA(i)
                if 1 <= i <= N:
                    stageB(i - 1)
                if 2 <= i <= N + 1:
                    stageB2(i - 2)
                if i >= 3:
                    stageC(i - 3)
                    hh, gg, kk = units[i - 3]
                    if gg == 3 and kk == 0 and hh + 4 < 8:
                        load_head(hh + 4)
            P.barrier()
            A.reset(mk)

        if upto >= 4:
            mla_attn()
        if upto >= 5:
            sb_attn()
            if debug:
                P.dma("pool", dbg_attn[:, 0:8, :], OT.ap, reads=[(OT, h, g) for h in range(8) for g in range(4)], writes=["dbg_attn"])
                P.dma("pool", dbg_attn[:, 8:16, :], SBT.ap, reads=[(SBT, h, g) for h in range(8) for g in range(4)], writes=["dbg_attn2"])
                P.barrier()

        def merge_out():
            mk = A.mark()
            mg = A.alloc("mg", [16, TOWN], BF16)
            bstg = Rot([A.alloc("bstg", [16, 128]) for _ in range(3)])
            wbm = Rot([A.alloc("wbm", [8, 128], BF16) for _ in range(2)])
            wbs = Rot([A.alloc("wbs", [8, 128], BF16) for _ in range(2)])
            gts = Rot([A.alloc("gts", [512]) for _ in range(4)])
            ms = Rot([A.alloc("ms", [512]) for _ in range(4)])
            allOT = [(OT, h, g) for h in range(8) for g in range(4)]
            allSB = [(SBT, h, g) for h in range(8) for g in range(4)]
            for dc in range(16):
                wa = wbm.next(); wb_ = wbs.next()
                load_wblk(w_bm, dc * 128, wa, 8, bstg)
                load_wblk(w_bs, dc * 128, wb_, 8, bstg)
                for g in range(4):
                    qc = slice(g * 512, (g + 1) * 512)
                    pa = psrot.next(); pb = psrot.next()
                    for h in range(8):
                        mm(pa.ap, wa[:, h, :], OT[:, h, qc], h == 0, h == 7, [(wa, h), (OT, h, g)], [pa])
                    for h in range(8):
                        mm(pb.ap, wb_[:, h, :], SBT[:, h, qc], h == 0, h == 7, [(wb_, h), (SBT, h, g)], [pb])
                    ga = gts.next(); gb = gts.next()
                    P.dma("sp", ga.ap, gaT[dc * 128:(dc + 1) * 128, qc], writes=[ga])
                    P.dma("sp", gb.ap, gbT[dc * 128:(dc + 1) * 128, qc], writes=[gb])
                    m1 = ms.next(); m2 = ms.next()
                    tt("dve", m1.ap, pa.ap, ga.ap, ALU.mult, [pa, ga], [m1])
                    tt("dve", m2.ap, pb.ap, gb.ap, ALU.mult, [pb, gb], [m2])
                    tt("dve", mg[:, dc, qc], m1.ap, m2.ap, ALU.add, [m1, m2], [(mg, dc, g)])
            wo = Rot([A.alloc("wo", [16, 128], BF16) for _ in range(3)])
            xs = Rot([A.alloc("xs", [512]) for _ in range(3)])
            x1s = Rot([A.alloc("x1s", [512]) for _ in range(3)])
            for dc in range(16):
                w_ = wo.next()
                load_wblk(w_out, dc * 128, w_, 16, bstg)
                for g in range(4):
                    qc = slice(g * 512, (g + 1) * 512)
                    ps = psrot.next()
                    for kc in range(16):
                        mm(ps.ap, w_[:, kc, :], mg[:, kc, qc], kc == 0, kc == 15, [(w_, kc), (mg, kc, g)], [ps])
                    xk = xs.next()
                    P.dma("sp", xk.ap, xT_own[dc * 128:(dc + 1) * 128, qc], writes=[xk])
                    x1 = x1s.next()
                    stt(x1.ap, ps.ap, GT1[:, dc:dc + 1], xk.ap, ALU.mult, ALU.add, [ps, xk, "pers"], [x1])
                    P.dma("pool", x1T[dc * 128:(dc + 1) * 128, qc], x1.ap, reads=[x1], writes=["x1T"])
            P.barrier()
            A.reset(mk)

        if upto >= 6:
            merge_out()
        A.reset(attn_mark)

        def peer():
            mk = A.mark()
            h2T = A.alloc("h2T", [16, TOWN], BF16)
            norm_tokens(x1T, 0, TOWN, A2, B2, 16, dst_buf=h2T)
            if debug:
                P.dma("pool", dbg_h2, h2T.ap, reads=[(h2T, kc, tg) for kc in range(16) for tg in range(4)], writes=["dbg_h2"])
            P.dma("pool", h2s, h2T.ap, reads=[], writes=["h2s"])
            P.barrier()
            mk2 = A.mark()
            bstg = Rot([A.alloc("bstg", [16, 128]) for _ in range(3)])
            wo = Rot([A.alloc("wo", [16, 128], BF16) for _ in range(2)])
            ost = Rot([A.alloc("ost", [512]) for _ in range(4)])
            for dc in range(16):
                w_ = wo.next()
                load_wblk(w_pq, dc * 128, w_, 16, bstg)
                for g in range(4):
                    qc = slice(g * 512, (g + 1) * 512)
                    ps = psrot.next()
                    for kc in range(16):
                        mm(ps.ap, w_[:, kc, :], h2T[:, kc, qc], kc == 0, kc == 15, [(w_, kc), (h2T, kc, g)], [ps])
                    o = ost.next()
                    cast(o.ap, ps.ap, [ps], [o])
                    P.dma("pool", pqT[dc * 128:(dc + 1) * 128, qc], o.ap, reads=[o], writes=["pqT"])
            P.barrier()
            A.reset(mk2)
            if upto < 8:
                return
            A.reset(mk)
            k1 = A.alloc("k1", [8, 128]); k2 = A.alloc("k2", [8, 128])
            iot = A.alloc("iot", [128])
            P.dma("sp", k1.ap, k1T, writes=[k1])
            P.dma("sp", k2.ap, k2T, writes=[k2])
            P.dma("sp", iot.ap, iota_d, writes=[iot])
            pqs = Rot([A.alloc("pq", [16, 128]) for _ in range(1)])
            s1 = A.alloc("s1", [8, 128]); s2 = A.alloc("s2", [8, 128])
            v1 = A.alloc("v1", [8, 16]); v2 = A.alloc("v2", [8, 16])
            idxu_b = A.alloc("idxu", [8, 16]); idxu = Buf(idxu_b.name, idxu_b.ap.bitcast(U32))
            idxf = A.alloc("idxf", [8, 16]); idxT = A.alloc("idxT", [128])
            mrc = A.alloc("mrc", [8, 256]); mr1 = Buf(mrc.name, mrc[:, :, 0:128]); mr2 = Buf(mrc.name, mrc[:, :, 128:256])
            cand = A.alloc("cand", [8, 256]); c16 = A.alloc("c16", [8, 16]); e16 = A.alloc("e16", [8, 16])
            negM = A.alloc("negM", [8]); Z = A.alloc("Z", [8]); lnZ = A.alloc("lnZ", [8]); nlse = A.alloc("nlse", [8]); thr = A.alloc("thr", [8])
            args = Rot([A.alloc("arg", [128, 16]) for _ in range(2)])
            Es = Rot([A.alloc("E", [128, 16], BF16) for _ in range(2)])
            R2 = A.alloc("R2", [128, 128], BF16)
            RT = A.alloc("RT", [128, 128], BF16)
            Pm = A.alloc("Pm", [128, 128], BF16)
            WTs = A.alloc("WTs", [128, 128], BF16)
            pqv = pqT.rearrange("(c p) t -> p c t", p=128)
            NEG = -1.0e30
            trot = Rot([psb[4], psb[5], psb[6], psb[7]])
            for tt_ in range(16):
                tc_ = slice(tt_ * 128, (tt_ + 1) * 128)
                pq = pqs.next()
                P.dma("sp", pq.ap, pqv[:, :, tc_], writes=[pq])
                for h in range(8):
                    mm(psb[0][:, h * 128:(h + 1) * 128] if h < 4 else psb[1][:, (h - 4) * 128:(h - 3) * 128],
                       pq[:, 2 * h, :], k1[:, h, :], True, True, [pq, k1], [psb[0] if h < 4 else psb[1]])
                for h in range(8):
                    mm(psb[2][:, h * 128:(h + 1) * 128] if h < 4 else psb[3][:, (h - 4) * 128:(h - 3) * 128],
                       pq[:, 2 * h + 1, :], k2[:, h, :], True, True, [pq, k2], [psb[2] if h < 4 else psb[3]])
                cp("act", s1[:, 0:4, :], psb[0].ap.rearrange("p (a b) -> p a b", a=4), [psb[0]], [s1])
                cp("act", s1[:, 4:8, :], psb[1].ap.rearrange("p (a b) -> p a b", a=4), [psb[1]], [s1])
                cp("dve", s2[:, 0:4, :], psb[2].ap.rearrange("p (a b) -> p a b", a=4), [psb[2]], [s2])
                cp("dve", s2[:, 4:8, :], psb[3].ap.rearrange("p (a b) -> p a b", a=4), [psb[3]], [s2])
                for h in range(8):
                    P.op("dve", lambda e, o=v1[:, h, 0:8], i=s1[:, h, :]: e.max(out=o, in_=i), reads=[s1], writes=[(v1, h, 0)])
                for h in range(8):
                    P.op("dve", lambda e, o=v2[:, h, 0:8], i=s2[:, h, :]: e.max(out=o, in_=i), reads=[s2], writes=[(v2, h, 0)])
                for h in range(8):
                    P.op("dve", lambda e, o=idxu[:, h, 0:8], m=v1[:, h, 0:8], i=s1[:, h, :]: e.max_index(out=o, in_max=m, in_values=i), reads=[s1, (v1, h, 0)], writes=[(idxu, h, 0)])
                for h in range(8):
                    P.op("dve", lambda e, o=mr1[:, h, :], r=v1[:, h, 0:8], i=s1[:, h, :]: e.match_replace(out=o, in_to_replace=r, in_values=i, imm_value=NEG), reads=[s1, (v1, h, 0)], writes=[(mrc, h, 0)])
                for h in range(8):
                    P.op("dve", lambda e, o=mr2[:, h, :], r=v2[:, h, 0:8], i=s2[:, h, :]: e.match_replace(out=o, in_to_replace=r, in_values=i, imm_value=NEG), reads=[s2, (v2, h, 0)], writes=[(mrc, h, 1)])
                for h in range(8):
                    P.op("dve", lambda e, o=v1[:, h, 8:16], i=mr1[:, h, :]: e.max(out=o, in_=i), reads=[(mrc, h, 0)], writes=[(v1, h, 1)])
                for h in range(8):
                    P.op("dve", lambda e, o=v2[:, h, 8:16], i=mr2[:, h, :]: e.max(out=o, in_=i), reads=[(mrc, h, 1)], writes=[(v2, h, 1)])
                for h in range(8):
                    P.op("dve", lambda e, o=idxu[:, h, 8:16], m=v1[:, h, 8:16], i=mr1[:, h, :]: e.max_index(out=o, in_max=m, in_values=i), reads=[(mrc, h, 0), (v1, h, 1)], writes=[(idxu, h, 1)])
                v1all = [(v1, h, k) for h in range(8) for k in range(2)]
                v2all = [(v2, h, k) for h in range(8) for k in range(2)]
                idxall = [(idxu, h, k) for h in range(8) for k in range(2)]
                cp("dve", idxf.ap, idxu.ap, idxall, [idxf])
                tt("dve", cand.ap.rearrange("p h (i j) -> p h i j", i=16),
                   v1.ap.unsqueeze(3).broadcast_to([128, 8, 16, 16]), v2.ap.unsqueeze(2).broadcast_to([128, 8, 16, 16]), ALU.add, v1all + v2all, [cand])
                for h in range(8):
                    P.op("dve", lambda e, o=c16[:, h, 0:8], i=cand[:, h, :]: e.max(out=o, in_=i), reads=[cand], writes=[(c16, h, 0)])
                for h in range(8):
                    P.op("dve", lambda e, o=mrc[:, h, :], r=c16[:, h, 0:8], i=cand[:, h, :]: e.match_replace(out=o, in_to_replace=r, in_values=i, imm_value=NEG), reads=[cand, (c16, h, 0)], writes=[(mrc, h, 0), (mrc, h, 1)])
                for h in range(8):
                    P.op("dve", lambda e, o=c16[:, h, 8:16], i=mrc[:, h, :]: e.max(out=o, in_=i), reads=[(mrc, h, 0), (mrc, h, 1)], writes=[(c16, h, 1)])
                c16all = [(c16, h, k) for h in range(8) for k in range(2)]
                ts("dve", negM.ap, c16[:, :, 0], -1.0, None, ALU.mult, None, c16all, [negM])
                cp("dve", thr.ap, c16[:, :, 15], c16all, [thr])
                tt("dve", e16.ap, c16.ap, negM.ap.unsqueeze(2).broadcast_to([128, 8, 16]), ALU.add, c16all + [negM], [e16])
                act(e16.ap, e16.ap, AF.Exp, [e16], [e16])
                P.op("dve", lambda e, o=Z.ap, i=e16.ap: e.tensor_reduce(out=o, in_=i, axis=AX.X, op=ALU.add), reads=[e16], writes=[Z])
                act(lnZ.ap, Z.ap, AF.Ln, [Z], [lnZ])
                tt("dve", nlse.ap, negM.ap, lnZ.ap, ALU.subtract, [negM, lnZ], [nlse])
                ib = trot.next()
                mm(ib[:, 0:128], idxf.ap.rearrange("p h i -> p (h i)"), ident_f.ap, True, True, [idxf, ident_f], [ib])
                cp("act", idxT.ap, ib[:, 0:128], [ib], [idxT])
                P.op("dve", lambda e: e.tensor_tensor(out=Pm.ap, in0=iot.ap.unsqueeze(1).broadcast_to([128, 128, 128]),
                                                       in1=idxT.ap.unsqueeze(2).broadcast_to([128, 128, 128]), op=ALU.is_equal), reads=[iot, idxT], writes=[Pm])
                rb = {}
                for k in range(9):
                    if k < 8:
                        h = k
                        a_ = args.next()
                        tt("dve", a_.ap, s2[:, h, :].unsqueeze(2).broadcast_to([128, 128, 16]),
                           v1[:, h, :].unsqueeze(1).broadcast_to([128, 128, 16]), ALU.add, [s2] + v1all, [a_])
                        E_ = Es.next()
                        act(E_.ap, a_.ap, AF.Exp, [a_, nlse], [E_], bias=nlse[:, h:h + 1], scale=1.0)
                        rb[h] = (a_, E_)
                    if k >= 1:
                        h = k - 1
                        a_, E_ = rb.pop(h)
                        stt(R2[:, :, h * 16:(h + 1) * 16], a_.ap, thr[:, h:h + 1], E_.ap, ALU.is_ge, ALU.mult, [a_, thr, E_], [(R2, h)])
                r2all = [(R2, h) for h in range(8)]
                for e4 in range(32):
                    b_ = trot.next()
                    for sl in range(4):
                        e2_ = e4 * 4 + sl
                        mm(b_[:, sl * 128:(sl + 1) * 128], R2[:, e2_, :], ident_b.ap, True, True, r2all + [ident_b], [b_])
                    cp("act", RT[:, :, e4 * 4:(e4 + 1) * 4], b_.ap.rearrange("p (e t) -> p t e", e=4), [b_], [(RT, e4)])
                rtall = [(RT, e4) for e4 in range(32)]
                for t4 in range(32):
                    b_ = trot.next()
                    for sl in range(4):
                        t_ = t4 * 4 + sl
                        mm(b_[:, sl * 128:(sl + 1) * 128], RT[:, t_, :], Pm[:, t_, :], True, True, rtall + [Pm], [b_])
                    cp("act", WTs[:, :, t4 * 4:(t4 + 1) * 4], b_.ap.rearrange("p (t e) -> p e t", t=4), [b_], [(WTs, t4)])
                wtall = [(WTs, t4) for t4 in range(32)]
                for q4 in range(4):
                    P.dma("pool", WT[q4 * 32:(q4 + 1) * 32, :, tc_].rearrange("a p t -> p a t"), WTs[:, q4 * 32:(q4 + 1) * 32, :], reads=wtall, writes=["WT"])
            P.barrier()
            A.reset(mk)
            if upto < 9:
                return
            h2T = A.alloc("h2T", [16, TOWN], BF16)
            P.dma("sp", h2T.ap, h2s, writes=[h2T])
            P.barrier()
            ust = Rot([A.alloc("ust", [16, 128]) for _ in range(4)])
            ub = Rot([A.alloc("ub", [16, 128], BF16) for _ in range(3)])
            wtb = Rot([A.alloc("wtb", [TOWN], BF16) for _ in range(3)])
            gl = Rot([A.alloc("gl", [512], BF16) for _ in range(3)])
            atb = Rot([A.alloc("atb", [TOWN], BF16) for _ in range(3)])
            uv = uT.rearrange("(kc p) e -> p kc e", p=128)
            pssets = [psb[0:4], psb[4:8]]
            for e1 in range(128):
                us = ust.next(); u_ = ub.next(); wt_ = wtb.next(); at_ = atb.next()
                P.dma("sp", us.ap, uv[:, :, e1 * 128:(e1 + 1) * 128], writes=[us])
                P.dma("sp", wt_.ap, WT[e1], writes=[wt_])
                cast(u_.ap, us.ap, [us], [u_])
                bset = pssets[e1 % 2]
                for kc in range(16):
                    for g in range(4):
                        qc = slice(g * 512, (g + 1) * 512)
                        mm(bset[g].ap, u_[:, kc, :], h2T[:, kc, qc], kc == 0, kc == 15, [u_, (h2T, kc, g)], [bset[g]])
                for g in range(4):
                    qc = slice(g * 512, (g + 1) * 512)
                    g_ = gl.next()
                    act(g_.ap, bset[g].ap, AF.Gelu, [bset[g]], [g_])
                    tt("dve", at_[:, qc], g_.ap, wt_[:, qc], ALU.mult, [g_, wt_], [(at_, g)])
                P.dma("pool", AT[e1], at_.ap, reads=[(at_, g) for g in range(4)], writes=["AT"])
            P.barrier()
            A.reset(mk)
            if upto < 10:
                return
            accb = A.alloc("accb", [16, TOWN])
            pssets = [psb[0:4], psb[4:8]]
            mkc = A.mark()
            vst = Rot([A.alloc("vst", [D]) for _ in range(1)])
            G = 4
            vbs = Rot([A.alloc("vb", [G, D], BF16) for _ in range(2)])
            abs_ = Rot([A.alloc("ab", [G, TOWN], BF16) for _ in range(2)])
            for eg in range(128 // G):
                vb = vbs.next(); ab = abs_.next()
                for l in range(G):
                    e1 = eg * G + l
                    vs = vst.next()
                    P.dma("sp", vs.ap, v_tab[e1 * 128:(e1 + 1) * 128, :], writes=[vs])
                    cast(vb[:, l, :], vs.ap, [vs], [(vb, l)])
                    P.dma("sp", ab[:, l, :], AT[e1], writes=[(ab, l)])
                for dc in range(16):
                    bset = pssets[dc % 2]
                    for l in range(G):
                        for g in range(4):
                            qc = slice(g * 512, (g + 1) * 512)
                            mm(bset[g].ap, vb[:, l, dc * 128:(dc + 1) * 128], ab[:, l, qc], l == 0, l == G - 1, [(vb, l), (ab, l)], [bset[g]])
                    for g in range(4):
                        qc = slice(g * 512, (g + 1) * 512)
                        if eg == 0:
                            cp("dve", accb[:, dc, qc], bset[g].ap, [bset[g]], [(accb, dc, g)])
                        else:
                            tt("dve", accb[:, dc, qc], accb[:, dc, qc], bset[g].ap, ALU.add, [(accb, dc, g), bset[g]], [(accb, dc, g)])
            if debug:
                P.dma("pool", dbg_peer, accb.ap, reads=[(accb, dc, g) for dc in range(16) for g in range(4)], writes=["dbg_peer"])
            P.barrier()
            A.reset(mkc)
            allacc = []
            xs = Rot([A.alloc("xs", [512]) for _ in range(3)])
            sqs = Rot([A.alloc("fsq", [512]) for _ in range(2)])
            r1 = A.alloc("fr1", [512]); rstd = A.alloc("frstd", [512])
            tms = Rot([A.alloc("ftm", [512]) for _ in range(3)])
            ssq = psb[7]
            for g in range(4):
                qc = slice(g * 512, (g + 1) * 512)
                for dc in range(16):
                    xk = xs.next()
                    P.dma("sp", xk.ap, x1T[dc * 128:(dc + 1) * 128, qc], writes=[xk])
                    stt(accb[:, dc, qc], accb[:, dc, qc], GT2[:, dc:dc + 1], xk.ap, ALU.mult, ALU.add, [(accb, dc, g), xk, "pers"], [(accb, dc, g)])
                    sq = sqs.next()
                    act(sq.ap, accb[:, dc, qc], AF.Square, [(accb, dc, g)], [sq])
                    mm(ssq.ap, ones_f.ap, sq.ap, dc == 0, dc == 15, [sq, ones_f], [ssq])
                act(r1.ap, ssq.ap, AF.Sqrt, [ssq], [r1], scale=1.0 / D, bias=EPS)
                P.op("dve", lambda e, o=rstd.ap, i=r1.ap: e.reciprocal(out=o, in_=i), reads=[r1], writes=[rstd])
                for dc in range(16):
                    tm = tms.next()
                    tt("dve", tm.ap, accb[:, dc, qc], rstd.ap, ALU.mult, [(accb, dc, g), rstd], [tm])
                    act(tm.ap, tm.ap, AF.Identity, [tm, "pers"], [tm], scale=GF[:, dc:dc + 1])
                    P.dma("pool", outT[dc * 128:(dc + 1) * 128, qc], tm.ap, reads=[tm], writes=["outT"])
            P.barrier()

        if upto >= 7:
            peer()
        P.barrier()
        P.emit()
    return nc
_PROG_CACHE = {}


def _own_idx(p):
    return np.concatenate([np.arange((2 * i + p) * 128, (2 * i + p + 1) * 128) for i in range(16)])


def _masks(p):
    j = np.arange(8)[None, :, None, None]
    s = np.arange(128)[:, None, None, None]
    il = np.arange(4)[None, None, :, None]
    r = np.arange(128)[None, None, None, :]
    mla = (2 * j + s // 64) <= (2 * (2 * il + p) + r // 64)
    sb = (128 * j + s) < (128 * (2 * il + p) + r)
    return (np.ascontiguousarray(mla.reshape(128, 8, 512)).astype(np.float32),
            np.ascontiguousarray(sb.reshape(128, 8, 512)).astype(np.float32))


def _col(v, n):
    return np.ascontiguousarray(np.asarray(v, np.float32).reshape(n, 128).T)


def make_in_maps(inputs, big_tabs=True, cores=range(8)):
    f = lambda k: np.asarray(inputs[k])
    x = f("x").astype(np.float32, copy=False)
    c = f("c"); positions = f("positions").astype(np.int32, copy=False)
    w_in = f("w_in")[0]
    cq, ckv, kr, sbq, sbk, sbv_, ga, gb = np.split(w_in, np.cumsum([512, 256, 64, 1024, 1024, 1024, 2048, 2048])[:-1].tolist(), axis=1)
    kr_sw = np.concatenate([kr[:, 32:], kr[:, :32]], axis=1)
    w_kv = np.ascontiguousarray(np.concatenate([ckv, kr, kr_sw, sbk, sbv_], axis=1))
    w_q1 = np.ascontiguousarray(np.concatenate([cq, sbq, ga, gb], axis=1))
    wuq = f("mla_w_uq")[0].reshape(512, 8, 192)
    w_uq_n = np.ascontiguousarray(wuq[:, :, :128].reshape(512, 1024))
    rope = wuq[:, :, 128:]
    w_uq_r = np.ascontiguousarray(rope.reshape(512, 512))
    w_uq_rs = np.ascontiguousarray(np.concatenate([rope[:, :, 32:], rope[:, :, :32]], axis=2).reshape(512, 512))
    wukv = f("mla_w_ukv")[0].reshape(256, 8, 256)
    w_ukv_k = np.ascontiguousarray(wukv[:, :, :128].reshape(256, 1024))
    w_ukv_v = np.ascontiguousarray(wukv[:, :, 128:].reshape(256, 1024))
    sk = f("peer_sub_keys")[0]
    k1T = np.ascontiguousarray(sk[:, 0].transpose(2, 0, 1))
    k2T = np.ascontiguousarray(sk[:, 1].transpose(2, 0, 1))
    if big_tabs:
        uT = np.ascontiguousarray(f("peer_u")[0].T)
        v_tab = np.ascontiguousarray(f("peer_v")[0])
    else:
        uT = np.zeros((128, 128), np.float32); v_tab = np.zeros((128, 128), np.float32)
    inv_freq = (np.float32(10000.0) ** (-np.arange(0, 64, 2, dtype=np.float32) / np.float32(64))).astype(np.float32)
    invf2 = np.concatenate([inv_freq, inv_freq])[:, None].astype(np.float32)
    sgn = np.concatenate([-np.ones(32, np.float32), np.ones(32, np.float32)])[:, None]
    ident = np.eye(128, dtype=np.float32)
    tmat = np.tril(np.ones((128, 128), np.float32), 0)
    shared = dict(
        ada_w=np.ascontiguousarray(f("ada_w")[0]), ada_b_col=_col(f("ada_b")[0], 96),
        g1_col=_col(f("norm1_g")[0], 16), g2_col=_col(f("norm2_g")[0], 16), gf_col=_col(f("final_norm_g"), 16),
        w_kv=w_kv, w_q1=w_q1, qn_g=_col(f("mla_q_norm_g")[0], 4), kvn_g=_col(f("mla_kv_norm_g")[0], 2),
        w_uq_n=w_uq_n, w_uq_r=w_uq_r, w_uq_rs=w_uq_rs, w_ukv_k=w_ukv_k, w_ukv_v=w_ukv_v,
        w_bm=np.ascontiguousarray(f("w_branch_mla")[0]), w_bs=np.ascontiguousarray(f("w_branch_sb")[0]),
        w_out=np.ascontiguousarray(f("w_out")[0]), w_pq=np.ascontiguousarray(f("peer_w_q")[0]),
        k1T=k1T, k2T=k2T, umat=np.ascontiguousarray(np.triu(np.ones((128, 128), np.float32), 1)), iota=np.ascontiguousarray(np.broadcast_to(np.arange(128, dtype=np.float32)[None, :], (128, 128))), uT=uT, v_tab=v_tab, invf2=invf2, sgn=sgn, ident=ident, tmat=tmat)
    masks = [_masks(0), _masks(1)]
    maps = []
    for core in cores:
        b, p = core // 2, core % 2
        idx = _own_idx(p)
        m = dict(shared)
        m["xT_nat"] = np.ascontiguousarray(x[b].T)
        m["xT_own"] = np.ascontiguousarray(x[b][idx].T)
        m["pos_nat"] = np.ascontiguousarray(positions[b][None, :])
        m["pos_own"] = np.ascontiguousarray(positions[b][idx][None, :])
        m["c_col"] = _col(c[b], 16)
        m["mask_mla"], m["mask_sb"] = masks[p]
        maps.append(m)
    return maps


def kernel(**inputs):
    if "nc" not in _PROG_CACHE:
        _PROG_CACHE["nc"] = build_program()
    nc = _PROG_CACHE["nc"]
    in_maps = make_in_maps(inputs)
    res = run_bass_kernel_spmd(nc, in_maps, core_ids=list(range(8)))
    out = np.empty((4, SEQ, D), np.float32)
    for core in range(8):
        b, p = core // 2, core % 2
        out[b, _own_idx(p), :] = np.asarray(res.results[core]["outT"]).T
    return out
```

```python
import numpy as np
import concourse.bass as bass
import concourse.mybir as mybir
from concourse.bass_utils import run_bass_kernel_spmd
from contextlib import ExitStack

F32 = mybir.dt.float32
BF16 = mybir.dt.bfloat16
I32 = mybir.dt.int32
U32 = mybir.dt.uint32
AF = mybir.ActivationFunctionType
ALU = mybir.AluOpType
AX = mybir.AxisListType

ENGS = ("pe", "act", "dve", "pool", "sp")


class Op:
    __slots__ = ("eng", "fn", "deps", "sig", "seq", "is_dma", "dsem", "dval", "prev", "extra")

    def __init__(self, eng, fn):
        self.eng = eng
        self.fn = fn
        self.deps = []
        self.sig = False
        self.seq = 0
        self.is_dma = False
        self.dsem = None
        self.dval = 0
        self.prev = None
        self.extra = None


class Prog:
    def __init__(self, nc, stack, n_dma_sems=12):
        self.nc = nc
        self.ops = {e: [] for e in ENGS}
        self.last_w = {}
        self.readers = {}
        self.stack = stack
        self.EP = 30000
        self.esem = {e: [] for e in ENGS}
        self.nsem = 0
        self.dsems = {}
        for q in ("sp", "pool", "act"):
            self.dsems[q] = [[self._newsem(), 0, None] for i in range(n_dma_sems)]
        self.dcur = {"sp": 0, "pool": 0, "act": 0}
        self.all_dma = []

    def _newsem(self):
        self.nsem += 1
        return self.stack.enter_context(self.nc.semaphore("sm%d" % self.nsem))

    @staticmethod
    def _k(b):
        if isinstance(b, tuple):
            return tuple(Prog._k(x) for x in b)
        if isinstance(b, (str, int)):
            return b
        return b.name

    def _deps(self, op, reads, writes):
        reads = [self._k(r) for r in reads]
        writes = [self._k(w) for w in writes]
        deps = []
        for r in reads:
            w = self.last_w.get(r)
            if w is not None:
                deps.append(w)
        for w_ in writes:
            w = self.last_w.get(w_)
            if w is not None:
                deps.append(w)
            deps.extend(self.readers.get(w_, ()))
        seen = set()
        for d in deps:
            if d is op or id(d) in seen:
                continue
            seen.add(id(d))
            if d.eng == "pe" and op.eng == "pe" and not d.is_dma:
                continue
            op.deps.append(d)
            if not d.is_dma:
                d.sig = True
        for r in reads:
            self.readers.setdefault(r, []).append(op)
        for w_ in writes:
            self.last_w[w_] = op
            self.readers[w_] = []

    def op(self, eng, fn, reads=(), writes=()):
        o = Op(eng, fn)
        self._deps(o, reads, writes)
        self.ops[eng].append(o)
        return o

    def dma(self, q, out, in_, reads=(), writes=(), **kw):
        o = Op(q, None)
        o.is_dma = True
        o.extra = (out, in_, kw)
        lst = self.dsems[q]
        i = self.dcur[q]
        self.dcur[q] = (i + 1) % len(lst)
        ent = lst[i]
        o.prev = ent[2]
        if ent[1] + 16 > 60000:
            ent[0] = self._newsem()
            ent[1] = 0
        ent[1] += 16
        ent[2] = o
        o.dsem = ent[0]
        o.dval = ent[1]
        self._deps(o, reads, writes)
        self.ops[q].append(o)
        self.all_dma.append(o)
        return o

    def barrier(self):
        lasts = []
        for e in ENGS:
            for o in reversed(self.ops[e]):
                if not o.is_dma and o.fn is not None:
                    o.sig = True
                    lasts.append(o)
                    break
        dm = []
        for q in self.dsems:
            for ent in self.dsems[q]:
                if ent[2] is not None:
                    dm.append(ent[2])
        for e in ENGS:
            o = Op(e, None)
            o.deps = [l for l in lasts] + dm
            self.ops[e].append(o)
        self.last_w = {}
        self.readers = {}

    def emit(self):
        nc = self.nc
        for e in ENGS:
            n = 0
            for o in self.ops[e]:
                if o.sig:
                    n += 1
                    o.seq = n
            while len(self.esem[e]) * self.EP < n + 1:
                self.esem[e].append(self._newsem())
        prog = self

        def run(e, eng):
            waited = {}
            dwaited = {}
            for o in prog.ops[e]:
                if o.is_dma and o.prev is not None:
                    k = id(o.prev.dsem)
                    if dwaited.get(k, 0) < o.prev.dval:
                        eng.wait_ge(o.prev.dsem, o.prev.dval)
                        dwaited[k] = o.prev.dval
                for d in o.deps:
                    if d.is_dma:
                        k = id(d.dsem)
                        if dwaited.get(k, 0) < d.dval:
                            eng.wait_ge(d.dsem, d.dval)
                            dwaited[k] = d.dval
                    else:
                        if waited.get(d.eng, 0) < d.seq:
                            eng.wait_ge(prog.esem[d.eng][(d.seq - 1) // prog.EP], (d.seq - 1) % prog.EP + 1)
                            waited[d.eng] = d.seq
                if o.is_dma:
                    out, in_, kw = o.extra
                    eng.dma_start(out=out, in_=in_, **kw).then_inc(o.dsem, 16)
                elif o.fn is not None:
                    ins = o.fn(eng)
                    if o.sig:
                        ins.then_inc(prog.esem[e][(o.seq - 1) // prog.EP], 1)

        with nc.Block() as block:
            @block.tensor
            def _(eng):
                run("pe", eng)

            @block.scalar
            def _(eng):
                run("act", eng)

            @block.vector
            def _(eng):
                run("dve", eng)

            @block.gpsimd
            def _(eng):
                run("pool", eng)

            @block.sync
            def _(eng):
                run("sp", eng)
import math

D = 2048
NKC = 16
SEQ = 4096
TOWN = 2048
EPS = 1e-6
NEXP = 16384


class Buf:
    def __init__(self, name, ap):
        self.name = name
        self.ap = ap

    def __getitem__(self, idx):
        return self.ap[idx]


class Arena:
    def __init__(self, big, nwords):
        self.big = big
        self.n = nwords
        self.off = 0
        self.uid = 0

    def mark(self):
        return self.off

    def reset(self, m):
        self.off = m

    def alloc(self, name, shape, dt=F32, parts=128):
        nel = 1
        for s in shape:
            nel *= s
        esz = 2 if dt == BF16 else 4
        nw = (nel * esz + 3) // 4
        nw = (nw + 15) // 16 * 16
        assert self.off + nw <= self.n, ("SBUF arena overflow", name, self.off, nw, self.n)
        v = self.big[0:parts, self.off:self.off + nw]
        self.off += nw
        if dt != F32:
            v = v.bitcast(dt)
        v = v[:, 0:nel]
        if len(shape) == 2:
            v = v.rearrange("p (a b) -> p a b", a=shape[0])
        elif len(shape) == 3:
            v = v.rearrange("p (a b c) -> p a b c", a=shape[0], b=shape[1])
        self.uid += 1
        return Buf("%s#%d" % (name, self.uid), v)


class Rot:
    def __init__(self, bufs):
        self.bufs = bufs
        self.i = 0

    def next(self):
        b = self.bufs[self.i % len(self.bufs)]
        self.i += 1
        return b


def build_program(upto=99, debug=False, dbg_names=()):
    nc = bass.Bass("TRN2", target_bir_lowering=False)

    def din(name, shape, dt=F32):
        return nc.dram_tensor(name, list(shape), dt, kind="ExternalInput").ap()

    def dscr(name, shape, dt=F32):
        kind = "ExternalOutput" if (debug and name in dbg_names) else "Internal"
        return nc.dram_tensor(name, list(shape), dt, kind=kind).ap()

    xT_nat = din("xT_nat", [D, SEQ])
    xT_own = din("xT_own", [D, TOWN])
    pos_nat = din("pos_nat", [1, SEQ], I32)
    pos_own = din("pos_own", [1, TOWN], I32)
    c_col = din("c_col", [128, 16])
    ada_w = din("ada_w", [D, 6 * D])
    ada_b_col = din("ada_b_col", [128, 96])
    g1_col = din("g1_col", [128, 16])
    g2_col = din("g2_col", [128, 16])
    gf_col = din("gf_col", [128, 16])
    w_kv = din("w_kv", [D, 2432])
    w_q1 = din("w_q1", [D, 5632])
    qn_g = din("qn_g", [128, 4])
    kvn_g = din("kvn_g", [128, 2])
    w_uq_n = din("w_uq_n", [512, 1024])
    w_uq_r = din("w_uq_r", [512, 512])
    w_uq_rs = din("w_uq_rs", [512, 512])
    w_ukv_k = din("w_ukv_k", [256, 1024])
    w_ukv_v = din("w_ukv_v", [256, 1024])
    w_bm = din("w_bm", [1024, D])
    w_bs = din("w_bs", [1024, D])
    w_out = din("w_out", [D, D])
    w_pq = din("w_pq", [D, D])
    k1T = din("k1T", [128, 8, 128])
    k2T = din("k2T", [128, 8, 128])
    big_tabs = upto >= 9
    uT = din("uT", [D, NEXP] if big_tabs else [128, 128])
    v_tab = din("v_tab", [NEXP, D] if big_tabs else [128, 128])
    invf2 = din("invf2", [64, 1])
    sgn = din("sgn", [64, 1])
    mask_mla = din("mask_mla", [128, 8, 512])
    mask_sb = din("mask_sb", [128, 8, 512])
    ident_d = din("ident", [128, 128])
    tmat_d = din("tmat", [128, 128])
    iota_d = din("iota", [128, 128])
    umat_d = din("umat", [128, 128])

    outT = nc.dram_tensor("outT", [D, TOWN], F32, kind="ExternalOutput").ap()

    ckvT = dscr("ckvT", [256, SEQ])
    krT = dscr("krT", [64, SEQ])
    krsT = dscr("krsT", [64, SEQ])
    sbkT = dscr("sbkT", [1024, SEQ], BF16)
    sbv = dscr("sbv", [SEQ, 1024], BF16)
    cqT = dscr("cqT", [512, TOWN])
    sbqT = dscr("sbqT", [1024, TOWN], BF16)
    gaT = dscr("gaT", [D, TOWN])
    gbT = dscr("gbT", [D, TOWN])
    KnT = dscr("KnT", [8, 128, SEQ], BF16)
    vmla = dscr("vmla", [SEQ, 1024], BF16)
    kropeT = dscr("kropeT", [64, SEQ], BF16)
    QnT = dscr("QnT", [8, 128, TOWN], BF16)
    QrT = dscr("QrT", [8, 64, TOWN], BF16)
    x1T = dscr("x1T", [D, TOWN])
    pqT = dscr("pqT", [D, TOWN])
    WT = dscr("WT", [128, 128, TOWN], BF16)
    AT = dscr("AT", [128, 128, TOWN], BF16)
    h2s = dscr("h2s", [128, 16, TOWN], BF16)
    dbg_mod = dscr("dbg_mod", [128, 96])
    dbg_attn = dscr("dbg_attn", [128, 16, TOWN], BF16)
    dbg_h2 = dscr("dbg_h2", [128, 16, TOWN], BF16)
    dbg_peer = dscr("dbg_peer", [128, 16, TOWN])

    with ExitStack() as st:
        P = Prog(nc, st, n_dma_sems=12)
        NW = 207 * 256
        big = st.enter_context(nc.sbuf_tensor("big", [128, NW], F32))
        psb = [Buf("ps%d" % i, st.enter_context(nc.psum_tensor("ps%d" % i, [128, 512], F32))[:]) for i in range(8)]
        A = Arena(big, NW)

        def mm(ps_ap, lhsT, rhs, start, stop, reads, writes, skip=False):
            if skip:
                return P.op("pe", lambda e: e.matmul(ps_ap, lhsT=lhsT, rhs=rhs, start=start, stop=stop, skip_group_check=True), reads=reads, writes=writes)
            return P.op("pe", lambda e: e.matmul(ps_ap, lhsT=lhsT, rhs=rhs, start=start, stop=stop), reads=reads, writes=writes)

        def act(out, in_, func, reads, writes, **kw):
            return P.op("act", lambda e: e.activation(out=out, in_=in_, func=func, **kw), reads=reads, writes=writes)

        def tt(eng, out, in0, in1, op, reads, writes):
            return P.op(eng, lambda e: e.tensor_tensor(out=out, in0=in0, in1=in1, op=op), reads=reads, writes=writes)

        def ts(eng, out, in0, s1, s2, op0, op1, reads, writes):
            if op1 is None:
                return P.op(eng, lambda e: e.tensor_scalar(out=out, in0=in0, scalar1=s1, scalar2=None, op0=op0), reads=reads, writes=writes)
            return P.op(eng, lambda e: e.tensor_scalar(out=out, in0=in0, scalar1=s1, scalar2=s2, op0=op0, op1=op1), reads=reads, writes=writes)

        def stt(out, in0, scalar, in1, op0, op1, reads, writes):
            return P.op("dve", lambda e: e.scalar_tensor_tensor(out=out, in0=in0, scalar=scalar, in1=in1, op0=op0, op1=op1), reads=reads, writes=writes)

        def cp(eng, out, in_, reads, writes):
            if eng == "act":
                return P.op("act", lambda e: e.copy(out=out, in_=in_), reads=reads, writes=writes)
            return P.op(eng, lambda e: e.tensor_copy(out=out, in_=in_), reads=reads, writes=writes)

        cast_i = [0]

        def cast(out, in_, reads, writes):
            cast_i[0] += 1
            return cp("dve" if cast_i[0] % 2 else "act", out, in_, reads, writes)

        ident_f = A.alloc("ident_f", [128])
        ident_b = A.alloc("ident_b", [128], BF16)
        ones_f = A.alloc("ones_f", [128])
        ones_b = A.alloc("ones_b", [128], BF16)
        tmat_f = A.alloc("tmat_f", [128])
        mod = A.alloc("mod", [96])
        A1 = A.alloc("A1", [16]); B1 = A.alloc("B1", [16]); A2 = A.alloc("A2", [16]); B2 = A.alloc("B2", [16])
        GT1 = A.alloc("GT1", [16]); GT2 = A.alloc("GT2", [16]); GF = A.alloc("GF", [16])
        g1s = A.alloc("g1s", [16]); g2s = A.alloc("g2s", [16])
        qng = A.alloc("qng", [4]); kvng = A.alloc("kvng", [2])
        invf = A.alloc("invf", [1], parts=64); sgnc = A.alloc("sgnc", [1], parts=64)
        PERS = ["pers"]

        P.dma("sp", ident_f.ap, ident_d, writes=[ident_f])
        P.dma("sp", tmat_f.ap, tmat_d, writes=[tmat_f])
        P.dma("sp", g1s.ap, g1_col, writes=[g1s])
        P.dma("sp", g2s.ap, g2_col, writes=[g2s])
        P.dma("sp", GF.ap, gf_col, writes=[GF])
        P.dma("sp", qng.ap, qn_g, writes=[qng])
        P.dma("sp", kvng.ap, kvn_g, writes=[kvng])
        P.dma("sp", invf.ap, invf2, writes=[invf])
        P.dma("sp", sgnc.ap, sgn, writes=[sgnc])
        cp("dve", ident_b.ap, ident_f.ap, [ident_f], [ident_b])
        P.op("dve", lambda e: e.memset(ones_f.ap, 1.0), writes=[ones_f])
        P.op("dve", lambda e: e.memset(ones_b.ap, 1.0), writes=[ones_b])
        m0 = A.mark()

        ccol = A.alloc("ccol", [16]); scol = A.alloc("scol", [16]); adab = A.alloc("adab", [96])
        P.dma("sp", ccol.ap, c_col, writes=[ccol])
        P.dma("sp", adab.ap, ada_b_col, writes=[adab])
        act(scol.ap, ccol.ap, AF.Silu, [ccol], [scol])
        scb = A.alloc("scb", [16], BF16)
        cp("dve", scb.ap, scol.ap, [scol], [scb])
        wst = Rot([A.alloc("adaw", [6 * D]) for _ in range(2)])
        wbb = Rot([A.alloc("adawb", [6 * D], BF16) for _ in range(2)])
        for kc in range(16):
            wj = wst.next(); wb_ = wbb.next()
            for q4 in range(4):
                cs = slice(q4 * 3072, (q4 + 1) * 3072)
                P.dma("sp", wj[:, cs], ada_w[kc * 128:(kc + 1) * 128, cs], writes=[(wj, q4)])
                cast(wb_[:, cs], wj[:, cs], [(wj, q4)], [(wb_, q4)])
            for j in range(96):
                P.op("pe", lambda e, o=psb[0][:, j:j + 1], l=wb_[:, j * 128:(j + 1) * 128], r=scb[:, kc:kc + 1], st=(kc == 0 and j == 0), sp=(kc == 15):
                     e.matmul(o, lhsT=l, rhs=r, start=st, stop=sp, skip_group_check=True), reads=[(wb_, j // 24), scb], writes=[psb[0]])
        tt("dve", mod.ap, psb[0][:, 0:96], adab.ap, ALU.add, [psb[0], adab], [mod])
        tmpc = A.alloc("tmpc", [16])
        ts("dve", tmpc.ap, mod[:, 16:32], 1.0, None, ALU.add, None, [mod], [tmpc])
        tt("dve", A1.ap, tmpc.ap, g1s.ap, ALU.mult, [tmpc, g1s], [A1])
        cp("dve", B1.ap, mod[:, 0:16], [mod], [B1])
        cp("dve", GT1.ap, mod[:, 32:48], [mod], [GT1])
        tmpc2 = A.alloc("tmpc2", [16])
        ts("dve", tmpc2.ap, mod[:, 64:80], 1.0, None, ALU.add, None, [mod], [tmpc2])
        tt("dve", A2.ap, tmpc2.ap, g2s.ap, ALU.mult, [tmpc2, g2s], [A2])
        cp("dve", B2.ap, mod[:, 48:64], [mod], [B2])
        cp("dve", GT2.ap, mod[:, 80:96], [mod], [GT2])
        if debug:
            P.dma("pool", dbg_mod, mod.ap, reads=[mod], writes=["dbg_mod"])
        P.barrier()
        A.reset(m0)

        def norm_tokens(src, t0, T, Acol, Bcol, ncs, dst_buf=None, dst_dram=None, inv_n=1.0 / D):
            srcv = src.rearrange("(kc p) t -> p kc t", p=128)
            nmk = A.mark()
            xbs = Rot([A.alloc("nx", [ncs, 512]) for _ in range(2)])
            sqs = Rot([A.alloc("nsq", [512]) for _ in range(2)])
            tms = Rot([A.alloc("ntm", [512]) for _ in range(3)])
            r1 = A.alloc("nr1", [512]); rstd = A.alloc("nrstd", [512])
            ssq = psb[7]
            for tg in range(T // 512):
                xb = xbs.next()
                P.dma("sp", xb.ap, srcv[:, :, t0 + tg * 512: t0 + (tg + 1) * 512], writes=[xb])
                for kc in range(ncs):
                    sq = sqs.next()
                    act(sq.ap, xb[:, kc, :], AF.Square, [xb], [sq])
                    mm(ssq.ap, ones_f.ap, sq.ap, kc == 0, kc == ncs - 1, [sq, ones_f], [ssq])
                act(r1.ap, ssq.ap, AF.Sqrt, [ssq], [r1], scale=inv_n, bias=EPS)
                P.op("dve", lambda e, o=rstd.ap, i=r1.ap: e.reciprocal(out=o, in_=i), reads=[r1], writes=[rstd])
                for kc in range(ncs):
                    tm = tms.next()
                    tt("dve", tm.ap, xb[:, kc, :], rstd.ap, ALU.mult, [xb, rstd], [tm])
                    if dst_buf is not None:
                        o = dst_buf[:, kc, tg * 512:(tg + 1) * 512]
                        kw = dict(scale=Acol[:, kc:kc + 1])
                        if Bcol is not None:
                            kw["bias"] = Bcol[:, kc:kc + 1]
                        act(o, tm.ap, AF.Identity, [tm, "pers"], [(dst_buf, kc, tg)], **kw)
                    else:
                        act(tm.ap, tm.ap, AF.Identity, [tm, "pers"], [tm], scale=Acol[:, kc:kc + 1])
                        P.dma("pool", dst_dram[kc * 128:(kc + 1) * 128, tg * 512:(tg + 1) * 512], tm.ap, reads=[tm], writes=["ndst"])
            P.barrier()
            A.reset(nmk)

        def load_w(wd, r0, c0, ncols, dst, nkc, stg):
            for kc in range(nkc):
                s = stg.next()
                P.dma("sp", s[:, 0:ncols], wd[r0 + kc * 128: r0 + (kc + 1) * 128, c0:c0 + ncols], writes=[s])
                cast(dst[:, kc, 0:ncols], s[:, 0:ncols], [s], [(dst, kc)])

        def load_wblk(wd, c0, dst, nkc, stg):
            s_ = stg.next()
            P.dma("sp", s_[:, 0:nkc, :], wd.rearrange("(kc p) n -> p kc n", p=128)[:, 0:nkc, c0:c0 + 128], writes=[s_])
            cast(dst.ap, s_[:, 0:nkc, :], [s_], [(dst, kc) for kc in range(nkc)])

        def wreads(dst, nkc):
            return [(dst, kc) for kc in range(nkc)]

        psrot = Rot([psb[0], psb[1], psb[2], psb[3]])

        def kv_phase():
            for half in range(2):
                mk = A.mark()
                hT = A.alloc("hT", [16, 2048], BF16)
                norm_tokens(xT_nat, half * 2048, 2048, A1, B1, 16, dst_buf=hT)
                hreads = [(hT, tg) for tg in range(4)]
                wstg = Rot([A.alloc("wstg", [1024]) for _ in range(3)])
                wbr = Rot([A.alloc("wb", [16, 1024], BF16) for _ in range(2)])
                ost = Rot([A.alloc("ost", [512]) for _ in range(4)])
                wb = wbr.next()
                load_w(w_kv, 0, 0, 384, wb, 16, wstg)
                for tg in range(4):
                    tcols = slice(tg * 512, (tg + 1) * 512)
                    gcols = slice(half * 2048 + tg * 512, half * 2048 + (tg + 1) * 512)
                    for blk in range(4):
                        ps = psrot.next()
                        if blk < 2:
                            c_lo, m = blk * 128, 128
                        else:
                            c_lo, m = 256 + (blk - 2) * 64, 64
                        for kc in range(16):
                            mm(ps[0:m, :], wb[:, kc, c_lo:c_lo + m], hT[:, kc, tcols], kc == 0, kc == 15, [(wb, kc), (hT, kc, tg)], [ps])
                        o = ost.next()
                        cast(o[0:m, :], ps[0:m, :], [ps], [o])
                        if blk < 2:
                            P.dma("pool", ckvT[blk * 128:(blk + 1) * 128, gcols], o.ap, reads=[o], writes=["ckvT"])
                        elif blk == 2:
                            P.dma("pool", krT[:, gcols], o[0:64, :], reads=[o], writes=["krT"])
                        else:
                            P.dma("pool", krsT[:, gcols], o[0:64, :], reads=[o], writes=["krsT"])
                obs = Rot([A.alloc("obs", [512], BF16) for _ in range(4)])
                wb = wbr.next()
                load_w(w_kv, 0, 384, 1024, wb, 16, wstg)
                for tg in range(4):
                    tcols = slice(tg * 512, (tg + 1) * 512)
                    gcols = slice(half * 2048 + tg * 512, half * 2048 + (tg + 1) * 512)
                    for blk in range(8):
                        ps = psrot.next()
                        for kc in range(16):
                            mm(ps.ap, wb[:, kc, blk * 128:(blk + 1) * 128], hT[:, kc, tcols], kc == 0, kc == 15, [(wb, kc), (hT, kc, tg)], [ps])
                        o = obs.next()
                        cast(o.ap, ps.ap, [ps], [o])
                        P.dma("pool", sbkT[blk * 128:(blk + 1) * 128, gcols], o.ap, reads=[o], writes=["sbkT"])
                wb = wbr.next()
                load_w(w_kv, 0, 1408, 1024, wb, 16, wstg)
                for tt_ in range(16):
                    tg = tt_ // 4
                    for cb in range(2):
                        ps = psrot.next()
                        for kc in range(16):
                            mm(ps.ap, hT[:, kc, tt_ * 128:(tt_ + 1) * 128], wb[:, kc, cb * 512:(cb + 1) * 512], kc == 0, kc == 15, [(wb, kc), (hT, kc, tg)], [ps])
                        o = obs.next()
                        cast(o.ap, ps.ap, [ps], [o])
                        r0 = half * 2048 + tt_ * 128
                        P.dma("pool", sbv[r0:r0 + 128, cb * 512:(cb + 1) * 512], o.ap, reads=[o], writes=["sbv"])
                P.barrier()
                A.reset(mk)

        if upto >= 1:
            kv_phase()

        def q_phase():
            mk = A.mark()
            hT = A.alloc("hT", [16, 2048], BF16)
            norm_tokens(xT_own, 0, 2048, A1, B1, 16, dst_buf=hT)
            wstg = Rot([A.alloc("wstg", [1024]) for _ in range(3)])
            wbr = Rot([A.alloc("wb", [16, 1024], BF16) for _ in range(2)])
            ost = Rot([A.alloc("ost", [512]) for _ in range(4)])
            obs = Rot([A.alloc("obs", [512], BF16) for _ in range(4)])
            groups = [(0, 512, "f32", cqT, 0), (512, 1024, "bf16", sbqT, 0),
                      (1536, 1024, "sig", gaT, 0), (2560, 1024, "sig", gaT, 1024),
                      (3584, 1024, "sig", gbT, 0), (4608, 1024, "sig", gbT, 1024)]
            for (c0, ncols, kind, dst, roff) in groups:
                wb = wbr.next()
                load_w(w_q1, 0, c0, ncols, wb, 16, wstg)
                for tg in range(4):
                    tcols = slice(tg * 512, (tg + 1) * 512)
                    for blk in range(ncols // 128):
                        ps = psrot.next()
                        for kc in range(16):
                            mm(ps.ap, wb[:, kc, blk * 128:(blk + 1) * 128], hT[:, kc, tcols], kc == 0, kc == 15, [(wb, kc), (hT, kc, tg)], [ps])
                        rows = slice(roff + blk * 128, roff + (blk + 1) * 128)
                        if kind == "bf16":
                            o = obs.next()
                            cast(o.ap, ps.ap, [ps], [o])
                        elif kind == "f32":
                            o = ost.next()
                            cast(o.ap, ps.ap, [ps], [o])
                        else:
                            o = ost.next()
                            act(o.ap, ps.ap, AF.Sigmoid, [ps], [o])
                        P.dma("pool", dst[rows, tcols], o.ap, reads=[o], writes=[("qd", id(dst))])
            P.barrier()
            A.reset(mk)

        if upto >= 2:
            q_phase()

        TWO_PI = 2.0 * math.pi
        C1 = 6.28125
        C2 = float(np.float32(TWO_PI - C1))
        MAGIC = 12582912.0

        def rope_tables(pos_d, t0, n, cos2, sin2, tmp):
            pi_, ang, kf, r = tmp[0], tmp[1], tmp[2], tmp[3]
            pint = tmp[4]
            P.dma("sp", pint[0:64, 0:n], pos_d[0:1, t0:t0 + n].broadcast_to([64, n]), writes=[pint])
            cp("dve", ang[0:64, 0:n], pint[0:64, 0:n], [pint], [ang])
            ts("dve", ang[0:64, 0:n], ang[0:64, 0:n], invf[0:64, 0:1], None, ALU.mult, None, [ang, "pers"], [ang])
            ts("dve", kf[0:64, 0:n], ang[0:64, 0:n], 1.0 / TWO_PI, MAGIC, ALU.mult, ALU.add, [ang], [kf])
            ts("dve", kf[0:64, 0:n], kf[0:64, 0:n], MAGIC, None, ALU.subtract, None, [kf], [kf])
            stt(r[0:64, 0:n], kf[0:64, 0:n], -C1, ang[0:64, 0:n], ALU.mult, ALU.add, [kf, ang], [r])
            stt(r[0:64, 0:n], kf[0:64, 0:n], -C2, r[0:64, 0:n], ALU.mult, ALU.add, [kf, r], [r])
            ts("dve", kf[0:64, 0:n], r[0:64, 0:n], -math.pi, TWO_PI, ALU.is_lt, ALU.mult, [r], [kf])
            tt("dve", r[0:64, 0:n], r[0:64, 0:n], kf[0:64, 0:n], ALU.add, [r, kf], [r])
            ts("dve", kf[0:64, 0:n], r[0:64, 0:n], math.pi, -TWO_PI, ALU.is_gt, ALU.mult, [r], [kf])
            tt("dve", r[0:64, 0:n], r[0:64, 0:n], kf[0:64, 0:n], ALU.add, [r, kf], [r])
            ts("dve", r[0:64, 0:n], r[0:64, 0:n], math.pi, -math.pi, ALU.min, ALU.max, [r], [r])
            act(pi_[0:64, 0:n], r[0:64, 0:n], AF.Sin, [r], [pi_])
            ts("dve", sin2, pi_[0:64, 0:n], sgnc[0:64, 0:1], None, ALU.mult, None, [pi_, "pers"], ["sin2"])
            ts("dve", kf[0:64, 0:n], r[0:64, 0:n], math.pi / 2, -TWO_PI, ALU.is_gt, ALU.mult, [r], [kf])
            tt("dve", kf[0:64, 0:n], kf[0:64, 0:n], r[0:64, 0:n], ALU.add, [kf, r], [kf])
            ts("dve", kf[0:64, 0:n], kf[0:64, 0:n], math.pi / 2, math.pi, ALU.add, ALU.min, [kf], [kf])
            act(cos2, kf[0:64, 0:n], AF.Sin, [kf], ["cos2"])

        def small_norm(src, T, ncs, gcol, dst, inv_n):
            norm_tokens(src, 0, T, gcol, None, ncs, dst_buf=dst, inv_n=inv_n)

        def mla_prep():
            mk = A.mark()
            kvnT = A.alloc("kvnT", [2, SEQ], BF16)
            small_norm(ckvT, SEQ, 2, kvng, kvnT, 1.0 / 256)
            qnT = A.alloc("qnT", [4, TOWN], BF16)
            small_norm(cqT, TOWN, 4, qng, qnT, 1.0 / 512)
            wstg = Rot([A.alloc("wstg", [1024]) for _ in range(3)])
            wk = A.alloc("wk", [2, 1024], BF16); wv = A.alloc("wv", [2, 1024], BF16)
            wn = A.alloc("wn", [4, 1024], BF16); wr = A.alloc("wr", [4, 512], BF16); wrs = A.alloc("wrs", [4, 512], BF16)
            load_w(w_ukv_k, 0, 0, 1024, wk, 2, wstg)
            load_w(w_ukv_v, 0, 0, 1024, wv, 2, wstg)
            load_w(w_uq_n, 0, 0, 1024, wn, 4, wstg)
            load_w(w_uq_r, 0, 0, 512, wr, 4, wstg)
            load_w(w_uq_rs, 0, 0, 512, wrs, 4, wstg)
            obs = Rot([A.alloc("obs", [512], BF16) for _ in range(4)])
            kvr = [(kvnT, tg) for tg in range(8)]
            for h in range(8):
                for tg in range(8):
                    ps = psrot.next()
                    for c in range(2):
                        mm(ps.ap, wk[:, c, h * 128:(h + 1) * 128], kvnT[:, c, tg * 512:(tg + 1) * 512], c == 0, c == 1, [(wk, c), (kvnT, c, tg)], [ps])
                    o = obs.next()
                    cast(o.ap, ps.ap, [ps], [o])
                    P.dma("pool", KnT[h, :, tg * 512:(tg + 1) * 512], o.ap, reads=[o], writes=["KnT"])
            for tt_ in range(32):
                for cb in range(2):
                    ps = psrot.next()
                    for c in range(2):
                        mm(ps.ap, kvnT[:, c, tt_ * 128:(tt_ + 1) * 128], wv[:, c, cb * 512:(cb + 1) * 512], c == 0, c == 1, [(wv, c), (kvnT, c, tt_ // 4)], [ps])
                    o = obs.next()
                    cast(o.ap, ps.ap, [ps], [o])
                    P.dma("pool", vmla[tt_ * 128:(tt_ + 1) * 128, cb * 512:(cb + 1) * 512], o.ap, reads=[o], writes=["vmla"])
            for h in range(8):
                for tg in range(4):
                    ps = psrot.next()
                    for c in range(4):
                        mm(ps.ap, wn[:, c, h * 128:(h + 1) * 128], qnT[:, c, tg * 512:(tg + 1) * 512], c == 0, c == 3, [(wn, c), (qnT, c, tg)], [ps])
                    o = obs.next()
                    cast(o.ap, ps.ap, [ps], [o])
                    P.dma("pool", QnT[h, :, tg * 512:(tg + 1) * 512], o.ap, reads=[o], writes=["QnT"])
            tmp = [A.alloc("rt%d" % i, [512]) for i in range(4)] + [Buf("pint", A.alloc("pint", [512]).ap.bitcast(I32))]
            cos2 = A.alloc("cos2", [512]); sin2 = A.alloc("sin2", [512])
            kx = Rot([A.alloc("kx", [512]) for _ in range(2)]); ky = Rot([A.alloc("ky", [512]) for _ in range(2)])
            m1 = A.alloc("m1", [512]); m2 = A.alloc("m2", [512])
            for tg in range(8):
                cols = slice(tg * 512, (tg + 1) * 512)
                rope_tables(pos_nat, tg * 512, 512, cos2[0:64, :], sin2[0:64, :], tmp)
                a = kx.next(); b = ky.next()
                P.dma("sp", a[0:64, :], krT[:, cols], writes=[a])
                P.dma("sp", b[0:64, :], krsT[:, cols], writes=[b])
                tt("dve", m1[0:64, :], a[0:64, :], cos2[0:64, :], ALU.mult, [a, "cos2"], [m1])
                tt("dve", m2[0:64, :], b[0:64, :], sin2[0:64, :], ALU.mult, [b, "sin2"], [m2])
                o = obs.next()
                tt("dve", o[0:64, :], m1[0:64, :], m2[0:64, :], ALU.add, [m1, m2], [o])
                P.dma("pool", kropeT[:, cols], o[0:64, :], reads=[o], writes=["kropeT"])
            for tg in range(4):
                cols = slice(tg * 512, (tg + 1) * 512)
                rope_tables(pos_own, tg * 512, 512, cos2[0:64, :], sin2[0:64, :], tmp)
                for h in range(8):
                    psa = psrot.next(); psc = psrot.next()
                    for c in range(4):
                        mm(psa[0:64, :], wr[:, c, h * 64:(h + 1) * 64], qnT[:, c, cols], c == 0, c == 3, [(wr, c), (qnT, c, tg)], [psa])
                    for c in range(4):
                        mm(psc[0:64, :], wrs[:, c, h * 64:(h + 1) * 64], qnT[:, c, cols], c == 0, c == 3, [(wrs, c), (qnT, c, tg)], [psc])
                    tt("dve", m1[0:64, :], psa[0:64, :], cos2[0:64, :], ALU.mult, [psa, "cos2"], [m1])
                    tt("dve", m2[0:64, :], psc[0:64, :], sin2[0:64, :], ALU.mult, [psc, "sin2"], [m2])
                    o = obs.next()
                    tt("dve", o[0:64, :], m1[0:64, :], m2[0:64, :], ALU.add, [m1, m2], [o])
                    P.dma("pool", QrT[h, :, cols], o[0:64, :], reads=[o], writes=["QrT"])
            P.barrier()
            A.reset(mk)

        if upto >= 3:
            mla_prep()

        attn_mark = A.mark()
        OT = A.alloc("OT", [8, TOWN], BF16)
        SBT = A.alloc("SBT", [8, TOWN], BF16)

        def mla_attn():
            mk = A.mark()
            scale = 192.0 ** -0.5
            mkf = A.alloc("mkf", [8, 512]); mkb = A.alloc("mkb", [8, 512], BF16)
            P.dma("sp", mkf.ap, mask_mla, writes=[mkf])
            cp("dve", mkb.ap, mkf.ap, [mkf], [mkb])
            kro = A.alloc("kro", [SEQ], BF16)
            P.dma("sp", kro[0:64, :], kropeT, writes=[kro])
            kn = Rot([A.alloc("kn", [SEQ], BF16) for _ in range(2)])
            vh = Rot([A.alloc("vh", [32, 128], BF16) for _ in range(2)])
            qn = Rot([A.alloc("qn", [TOWN], BF16) for _ in range(2)])
            qr = Rot([A.alloc("qr", [TOWN], BF16) for _ in range(2)])
            pts = Rot([A.alloc("pt", [512], BF16) for _ in range(3)])
            rden = A.alloc("rden", [512])
            sps = Rot([psb[0], psb[1], psb[2]])
            acc = psb[4]; den = psb[5]
            hb = {}

            def load_head(h):
                k_ = kn.next(); v_ = vh.next(); q_ = qn.next(); r_ = qr.next()
                P.dma("sp", k_.ap, KnT[h], writes=[k_])
                P.dma("sp", v_.ap, vmla[:, h * 128:(h + 1) * 128].rearrange("(kb p) d -> p kb d", p=128), writes=[v_])
                P.dma("sp", q_.ap, QnT[h], writes=[q_])
                P.dma("sp", r_[0:64, :], QrT[h], writes=[r_])
                hb[h] = (k_, v_, q_, r_)

            units = [(h, g, kb) for h in range(8) for g in range(4) for kb in range(8 * g + 8)]
            sbank = {}

            def stage1(n):
                h, g, kb = units[n]
                k_, v_, q_, r_ = hb[h]
                qc = slice(g * 512, (g + 1) * 512); kc_ = slice(kb * 128, (kb + 1) * 128)
                s_ = sps.next()
                sbank[n] = s_
                mm(s_.ap, k_[:, kc_], q_[:, qc], True, False, [k_, q_], [s_])
                mm(s_.ap, kro[0:64, kc_], r_[0:64, qc], False, True, [kro, r_], [s_])

            def stage2(n):
                h, g, kb = units[n]
                k_, v_, q_, r_ = hb[h]
                nkb = 8 * g + 8
                qc = slice(g * 512, (g + 1) * 512)
                s_ = sbank.pop(n)
                pt = pts.next()
                act(pt.ap, s_.ap, AF.Exp, [s_], [pt], scale=scale)
                j = kb - 8 * g
                if j >= 0:
                    tt("dve", pt.ap, pt.ap, mkb[:, j, :], ALU.mult, [pt, mkb], [pt])
                mm(acc.ap, v_[:, kb, :], pt.ap, kb == 0, kb == nkb - 1, [v_, pt], [acc])
                mm(den.ap, ones_b.ap, pt.ap, kb == 0, kb == nkb - 1, [ones_b, pt], [den])
                if kb == nkb - 1:
                    P.op("dve", lambda e, o=rden.ap, i=den.ap: e.reciprocal(out=o, in_=i), reads=[den], writes=[rden])
                    tt("dve", OT[:, h, qc], acc.ap, rden.ap, ALU.mult, [acc, rden], [(OT, h, g)])

            load_head(0)
            load_head(1)
            N = len(units)
            for n in range(N + 1):
                if n < N:
                    stage1(n)
                if n >= 1:
                    stage2(n - 1)
                    hh, gg, kk = units[n - 1]
                    if gg == 3 and kk == 31 and hh + 2 < 8:
                        load_head(hh + 2)
            P.barrier()
            A.reset(mk)

        def sb_attn():
            mk = A.mark()
            scale = 128.0 ** -0.5
            mkf = A.alloc("mkf", [8, 512]); mkb = A.alloc("mkb", [8, 512], BF16)
            P.dma("sp", mkf.ap, mask_sb, writes=[mkf])
            cp("dve", mkb.ap, mkf.ap, [mkf], [mkb])
            umf = A.alloc("umf", [128]); tmf = A.alloc("tmf", [128]); tmb = A.alloc("tmb", [128], BF16); umb = A.alloc("umb", [128], BF16)
            P.dma("sp", umf.ap, umat_d, writes=[umf])
            P.dma("sp", tmf.ap, tmat_d, writes=[tmf])
            cp("dve", umb.ap, umf.ap, [umf], [umb])
            cp("dve", tmb.ap, tmf.ap, [tmf], [tmb])
            kn = Rot([A.alloc("kn", [SEQ], BF16) for _ in range(4)])
            vh = Rot([A.alloc("vh", [32, 128], BF16) for _ in range(4)])
            qn = Rot([A.alloc("qn", [TOWN], BF16) for _ in range(4)])
            es = Rot([A.alloc("es", [512]) for _ in range(4)])
            ens = Rot([A.alloc("en", [512]) for _ in range(3)])
            sp_ = Rot([A.alloc("sp", [512]) for _ in range(3)])
            his = Rot([A.alloc("hi", [512], BF16) for _ in range(5)])
            los = Rot([A.alloc("lo", [512], BF16) for _ in range(5)])
            wts = Rot([A.alloc("wt", [512], BF16) for _ in range(3)])
            zps = Rot([psb[0], psb[1], psb[2], psb[3]])
            Lb = [psb[4], psb[5]]
            accs = [psb[6], psb[7]]
            hb = {}

            def load_head(h):
                k_ = kn.next(); v_ = vh.next(); q_ = qn.next()
                P.dma("sp", k_.ap, sbkT[h * 128:(h + 1) * 128, :], writes=[k_])
                P.dma("sp", v_.ap, sbv[:, h * 128:(h + 1) * 128].rearrange("(kb p) d -> p kb d", p=128), writes=[v_])
                P.dma("sp", q_.ap, sbqT[h * 128:(h + 1) * 128, :], writes=[q_])
                hb[h] = (k_, v_, q_)

            units = []
            for hp in range(4):
                for g in range(4):
                    for kb in range(8 * g + 7, -1, -1):
                        units.append((2 * hp, g, kb))
                        units.append((2 * hp + 1, g, kb))
            st_ = {}

            def stageA(n):
                h, g, kb = units[n]
                k_, v_, q_ = hb[h]
                qc = slice(g * 512, (g + 1) * 512); kc_ = slice(kb * 128, (kb + 1) * 128)
                z = zps.next()
                mm(z.ap, k_[:, kc_], q_[:, qc], True, True, [k_, q_], [z])
                st_[n] = [z]

            def stageB(n):
                h, g, kb = units[n]
                z = st_[n][0]
                e_ = es.next()
                act(e_.ap, z.ap, AF.Exp, [z], [e_], scale=scale)
                s_ = sp_.next()
                act(s_.ap, e_.ap, AF.Ln, [e_], [s_], bias=1.0, scale=1.0)
                j = kb - 8 * g
                if j >= 0:
                    tt("dve", s_.ap, s_.ap, mkf[:, j, :], ALU.mult, [s_, mkf], [s_])
                hi = his.next(); lo = los.next()
                cp("dve", hi.ap, s_.ap, [s_], [hi])
                tt("pool", lo.ap, s_.ap, hi.ap, ALU.subtract, [s_, hi], [lo])
                st_[n] = [z, hi, lo, None, e_]

            def stageB2(n):
                h, g, kb = units[n]
                nkb = 8 * g + 8
                first = (kb == nkb - 1)
                L = Lb[n % 2]
                z, hi, lo, _, e_ = st_[n]
                if not first:
                    phi, plo = st_[n - 2][1], st_[n - 2][2]
                    mm(L.ap, umb.ap, phi.ap, False, False, [umb, phi], [L], skip=True)
                    mm(L.ap, umb.ap, plo.ap, False, False, [umb, plo], [L], skip=True)
                mm(L.ap, tmb.ap, hi.ap, first, False, [tmb, hi], [L], skip=True)
                mm(L.ap, tmb.ap, lo.ap, False, kb == 0, [tmb, lo], [L], skip=True)

            def stageB3(n):
                L = Lb[n % 2]
                en = ens.next()
                act(en.ap, L.ap, AF.Exp, [L], [en], scale=-1.0)
                st_[n][3] = en

            def stageC(n):
                h, g, kb = units[n]
                k_, v_, q_ = hb[h]
                nkb = 8 * g + 8
                first = (kb == nkb - 1)
                qc = slice(g * 512, (g + 1) * 512)
                acc = accs[n % 2]
                en = st_[n][3]; e_ = st_[n][4]
                w_ = wts.next()
                tt("dve", w_.ap, e_.ap, en.ap, ALU.mult, [e_, en], [w_])
                j = kb - 8 * g
                if j >= 0:
                    tt("dve", w_.ap, w_.ap, mkb[:, j, :], ALU.mult, [w_, mkb], [w_])
                mm(acc.ap, v_[:, kb, :], w_.ap, first, kb == 0, [v_, w_], [acc])
                if kb == 0:
                    cp("act", SBT[:, h, qc], acc.ap, [acc], [(SBT, h, g)])
                if n >= 6:
                    st_.pop(n - 6, None)

            for h in range(4):
                load_head(h)
            N = len(units)
            for i in range(N + 4):
                if i < N:
                    stageA(i)
                if 2 <= i <= N + 1:
                    stageB2(i - 2)
                if i >= 4:
                    stageC(i - 4)
                if 1 <= i <= N:
                    stageB(i - 1)
                if 3 <= i <= N + 2:
                    stageB3(i - 3)
                if i >= 4:
                    hh, gg, kk = units[i - 4]
                    if gg == 3 and kk == 0 and hh + 4 < 8:
                        load_head(hh + 4)
            P.barrier()
            A.reset(mk)

        if upto >= 4:
            mla_attn()
        if upto >= 5:
            sb_attn()
            if debug:
                P.dma("pool", dbg_attn[:, 0:8, :], OT.ap, reads=[(OT, h, g) for h in range(8) for g in range(4)], writes=["dbg_attn"])
                P.dma("pool", dbg_attn[:, 8:16, :], SBT.ap, reads=[(SBT, h, g) for h in range(8) for g in range(4)], writes=["dbg_attn2"])
                P.barrier()

        def merge_out():
            mk = A.mark()
            mg = A.alloc("mg", [16, TOWN], BF16)
            bstg = Rot([A.alloc("bstg", [16, 128]) for _ in range(3)])
            wbm = Rot([A.alloc("wbm", [8, 128], BF16) for _ in range(2)])
            wbs = Rot([A.alloc("wbs", [8, 128], BF16) for _ in range(2)])
            gts = Rot([A.alloc("gts", [512]) for _ in range(4)])
            ms = Rot([A.alloc("ms", [512]) for _ in range(4)])
            allOT = [(OT, h, g) for h in range(8) for g in range(4)]
            allSB = [(SBT, h, g) for h in range(8) for g in range(4)]
            for dc in range(16):
                wa = wbm.next(); wb_ = wbs.next()
                load_wblk(w_bm, dc * 128, wa, 8, bstg)
                load_wblk(w_bs, dc * 128, wb_, 8, bstg)
                for g in range(4):
                    qc = slice(g * 512, (g + 1) * 512)
                    pa = psrot.next(); pb = psrot.next()
                    for h in range(8):
                        mm(pa.ap, wa[:, h, :], OT[:, h, qc], h == 0, h == 7, [(wa, h), (OT, h, g)], [pa])
                    for h in range(8):
                        mm(pb.ap, wb_[:, h, :], SBT[:, h, qc], h == 0, h == 7, [(wb_, h), (SBT, h, g)], [pb])
                    ga = gts.next(); gb = gts.next()
                    P.dma("sp", ga.ap, gaT[dc * 128:(dc + 1) * 128, qc], writes=[ga])
                    P.dma("sp", gb.ap, gbT[dc * 128:(dc + 1) * 128, qc], writes=[gb])
                    m1 = ms.next(); m2 = ms.next()
                    tt("dve", m1.ap, pa.ap, ga.ap, ALU.mult, [pa, ga], [m1])
                    tt("dve", m2.ap, pb.ap, gb.ap, ALU.mult, [pb, gb], [m2])
                    tt("dve", mg[:, dc, qc], m1.ap, m2.ap, ALU.add, [m1, m2], [(mg, dc, g)])
            wo = Rot([A.alloc("wo", [16, 128], BF16) for _ in range(3)])
            xs = Rot([A.alloc("xs", [512]) for _ in range(3)])
            x1s = Rot([A.alloc("x1s", [512]) for _ in range(3)])
            for dc in range(16):
                w_ = wo.next()
                load_wblk(w_out, dc * 128, w_, 16, bstg)
                for g in range(4):
                    qc = slice(g * 512, (g + 1) * 512)
                    ps = psrot.next()
                    for kc in range(16):
                        mm(ps.ap, w_[:, kc, :], mg[:, kc, qc], kc == 0, kc == 15, [(w_, kc), (mg, kc, g)], [ps])
                    xk = xs.next()
                    P.dma("sp", xk.ap, xT_own[dc * 128:(dc + 1) * 128, qc], writes=[xk])
                    x1 = x1s.next()
                    stt(x1.ap, ps.ap, GT1[:, dc:dc + 1], xk.ap, ALU.mult, ALU.add, [ps, xk, "pers"], [x1])
                    P.dma("pool", x1T[dc * 128:(dc + 1) * 128, qc], x1.ap, reads=[x1], writes=["x1T"])
            P.barrier()
            A.reset(mk)

        if upto >= 6:
            merge_out()
        A.reset(attn_mark)

        def peer():
            mk = A.mark()
            h2T = A.alloc("h2T", [16, TOWN], BF16)
            norm_tokens(x1T, 0, TOWN, A2, B2, 16, dst_buf=h2T)
            if debug:
                P.dma("pool", dbg_h2, h2T.ap, reads=[(h2T, kc, tg) for kc in range(16) for tg in range(4)], writes=["dbg_h2"])
            P.dma("pool", h2s, h2T.ap, reads=[], writes=["h2s"])
            P.barrier()
            mk2 = A.mark()
            bstg = Rot([A.alloc("bstg", [16, 128]) for _ in range(3)])
            wo = Rot([A.alloc("wo", [16, 128], BF16) for _ in range(2)])
            ost = Rot([A.alloc("ost", [512]) for _ in range(4)])
            for dc in range(16):
                w_ = wo.next()
                load_wblk(w_pq, dc * 128, w_, 16, bstg)
                for g in range(4):
                    qc = slice(g * 512, (g + 1) * 512)
                    ps = psrot.next()
                    for kc in range(16):
                        mm(ps.ap, w_[:, kc, :], h2T[:, kc, qc], kc == 0, kc == 15, [(w_, kc), (h2T, kc, g)], [ps])
                    o = ost.next()
                    cast(o.ap, ps.ap, [ps], [o])
                    P.dma("pool", pqT[dc * 128:(dc + 1) * 128, qc], o.ap, reads=[o], writes=["pqT"])
            P.barrier()
            A.reset(mk2)
            if upto < 8:
                return
            A.reset(mk)
            k1 = A.alloc("k1", [8, 128]); k2 = A.alloc("k2", [8, 128])
            iot = A.alloc("iot", [128])
            P.dma("sp", k1.ap, k1T, writes=[k1])
            P.dma("sp", k2.ap, k2T, writes=[k2])
            P.dma("sp", iot.ap, iota_d, writes=[iot])
            pqs = Rot([A.alloc("pq", [16, 128]) for _ in range(1)])
            s1 = A.alloc("s1", [8, 128]); s2 = A.alloc("s2", [8, 128])
            v1 = A.alloc("v1", [8, 16]); v2 = A.alloc("v2", [8, 16])
            idxu_b = A.alloc("idxu", [8, 16]); idxu = Buf(idxu_b.name, idxu_b.ap.bitcast(U32))
            idxf = A.alloc("idxf", [8, 16]); idxT = A.alloc("idxT", [128])
            mrc = A.alloc("mrc", [8, 256]); mr1 = Buf(mrc.name, mrc[:, :, 0:128]); mr2 = Buf(mrc.name, mrc[:, :, 128:256])
            cand = A.alloc("cand", [8, 256]); c16 = A.alloc("c16", [8, 16]); e16 = A.alloc("e16", [8, 16])
            negM = A.alloc("negM", [8]); Z = A.alloc("Z", [8]); lnZ = A.alloc("lnZ", [8]); nlse = A.alloc("nlse", [8]); thr = A.alloc("thr", [8])
            args = Rot([A.alloc("arg", [128, 16]) for _ in range(2)])
            Es = Rot([A.alloc("E", [128, 16], BF16) for _ in range(2)])
            R2 = A.alloc("R2", [128, 128], BF16)
            RT = A.alloc("RT", [128, 128], BF16)
            Pm = A.alloc("Pm", [128, 128], BF16)
            WTs = A.alloc("WTs", [128, 128], BF16)
            pqv = pqT.rearrange("(c p) t -> p c t", p=128)
            NEG = -1.0e30
            trot = Rot([psb[4], psb[5], psb[6], psb[7]])
            for tt_ in range(16):
                tc_ = slice(tt_ * 128, (tt_ + 1) * 128)
                pq = pqs.next()
                P.dma("sp", pq.ap, pqv[:, :, tc_], writes=[pq])
                for h in range(8):
                    mm(psb[0][:, h * 128:(h + 1) * 128] if h < 4 else psb[1][:, (h - 4) * 128:(h - 3) * 128],
                       pq[:, 2 * h, :], k1[:, h, :], True, True, [pq, k1], [psb[0] if h < 4 else psb[1]])
                for h in range(8):
                    mm(psb[2][:, h * 128:(h + 1) * 128] if h < 4 else psb[3][:, (h - 4) * 128:(h - 3) * 128],
                       pq[:, 2 * h + 1, :], k2[:, h, :], True, True, [pq, k2], [psb[2] if h < 4 else psb[3]])
                cp("act", s1[:, 0:4, :], psb[0].ap.rearrange("p (a b) -> p a b", a=4), [psb[0]], [s1])
                cp("act", s1[:, 4:8, :], psb[1].ap.rearrange("p (a b) -> p a b", a=4), [psb[1]], [s1])
                cp("dve", s2[:, 0:4, :], psb[2].ap.rearrange("p (a b) -> p a b", a=4), [psb[2]], [s2])
                cp("dve", s2[:, 4:8, :], psb[3].ap.rearrange("p (a b) -> p a b", a=4), [psb[3]], [s2])
                for h in range(8):
                    P.op("dve", lambda e, o=v1[:, h, 0:8], i=s1[:, h, :]: e.max(out=o, in_=i), reads=[s1], writes=[(v1, h, 0)])
                for h in range(8):
                    P.op("dve", lambda e, o=v2[:, h, 0:8], i=s2[:, h, :]: e.max(out=o, in_=i), reads=[s2], writes=[(v2, h, 0)])
                for h in range(8):
                    P.op("dve", lambda e, o=idxu[:, h, 0:8], m=v1[:, h, 0:8], i=s1[:, h, :]: e.max_index(out=o, in_max=m, in_values=i), reads=[s1, (v1, h, 0)], writes=[(idxu, h, 0)])
                for h in range(8):
                    P.op("dve", lambda e, o=mr1[:, h, :], r=v1[:, h, 0:8], i=s1[:, h, :]: e.match_replace(out=o, in_to_replace=r, in_values=i, imm_value=NEG), reads=[s1, (v1, h, 0)], writes=[(mrc, h, 0)])
                for h in range(8):
                    P.op("dve", lambda e, o=mr2[:, h, :], r=v2[:, h, 0:8], i=s2[:, h, :]: e.match_replace(out=o, in_to_replace=r, in_values=i, imm_value=NEG), reads=[s2, (v2, h, 0)], writes=[(mrc, h, 1)])
                for h in range(8):
                    P.op("dve", lambda e, o=v1[:, h, 8:16], i=mr1[:, h, :]: e.max(out=o, in_=i), reads=[(mrc, h, 0)], writes=[(v1, h, 1)])
                for h in range(8):
                    P.op("dve", lambda e, o=v2[:, h, 8:16], i=mr2[:, h, :]: e.max(out=o, in_=i), reads=[(mrc, h, 1)], writes=[(v2, h, 1)])
                for h in range(8):
                    P.op("dve", lambda e, o=idxu[:, h, 8:16], m=v1[:, h, 8:16], i=mr1[:, h, :]: e.max_index(out=o, in_max=m, in_values=i), reads=[(mrc, h, 0), (v1, h, 1)], writes=[(idxu, h, 1)])
                v1all = [(v1, h, k) for h in range(8) for k in range(2)]
                v2all = [(v2, h, k) for h in range(8) for k in range(2)]
                idxall = [(idxu, h, k) for h in range(8) for k in range(2)]
                cp("dve", idxf.ap, idxu.ap, idxall, [idxf])
                tt("dve", cand.ap.rearrange("p h (i j) -> p h i j", i=16),
                   v1.ap.unsqueeze(3).broadcast_to([128, 8, 16, 16]), v2.ap.unsqueeze(2).broadcast_to([128, 8, 16, 16]), ALU.add, v1all + v2all, [cand])
                for h in range(8):
                    P.op("dve", lambda e, o=c16[:, h, 0:8], i=cand[:, h, :]: e.max(out=o, in_=i), reads=[cand], writes=[(c16, h, 0)])
                for h in range(8):
                    P.op("dve", lambda e, o=mrc[:, h, :], r=c16[:, h, 0:8], i=cand[:, h, :]: e.match_replace(out=o, in_to_replace=r, in_values=i, imm_value=NEG), reads=[cand, (c16, h, 0)], writes=[(mrc, h, 0), (mrc, h, 1)])
                for h in range(8):
                    P.op("dve", lambda e, o=c16[:, h, 8:16], i=mrc[:, h, :]: e.max(out=o, in_=i), reads=[(mrc, h, 0), (mrc, h, 1)], writes=[(c16, h, 1)])
                c16all = [(c16, h, k) for h in range(8) for k in range(2)]
                ts("dve", negM.ap, c16[:, :, 0], -1.0, None, ALU.mult, None, c16all, [negM])
                cp("dve", thr.ap, c16[:, :, 15], c16all, [thr])
                tt("dve", e16.ap, c16.ap, negM.ap.unsqueeze(2).broadcast_to([128, 8, 16]), ALU.add, c16all + [negM], [e16])
                act(e16.ap, e16.ap, AF.Exp, [e16], [e16])
                P.op("dve", lambda e, o=Z.ap, i=e16.ap: e.tensor_reduce(out=o, in_=i, axis=AX.X, op=ALU.add), reads=[e16], writes=[Z])
                act(lnZ.ap, Z.ap, AF.Ln, [Z], [lnZ])
                tt("dve", nlse.ap, negM.ap, lnZ.ap, ALU.subtract, [negM, lnZ], [nlse])
                ib = trot.next()
                mm(ib[:, 0:128], idxf.ap.rearrange("p h i -> p (h i)"), ident_f.ap, True, True, [idxf, ident_f], [ib])
                cp("act", idxT.ap, ib[:, 0:128], [ib], [idxT])
                P.op("dve", lambda e: e.tensor_tensor(out=Pm.ap, in0=iot.ap.unsqueeze(1).broadcast_to([128, 128, 128]),
                                                       in1=idxT.ap.unsqueeze(2).broadcast_to([128, 128, 128]), op=ALU.is_equal), reads=[iot, idxT], writes=[Pm])
                rb = {}
                for k in range(9):
                    if k < 8:
                        h = k
                        a_ = args.next()
                        tt("dve", a_.ap, s2[:, h, :].unsqueeze(2).broadcast_to([128, 128, 16]),
                           v1[:, h, :].unsqueeze(1).broadcast_to([128, 128, 16]), ALU.add, [s2] + v1all, [a_])
                        E_ = Es.next()
                        act(E_.ap, a_.ap, AF.Exp, [a_, nlse], [E_], bias=nlse[:, h:h + 1], scale=1.0)
                        rb[h] = (a_, E_)
                    if k >= 1:
                        h = k - 1
                        a_, E_ = rb.pop(h)
                        stt(R2[:, :, h * 16:(h + 1) * 16], a_.ap, thr[:, h:h + 1], E_.ap, ALU.is_ge, ALU.mult, [a_, thr, E_], [(R2, h)])
                r2all = [(R2, h) for h in range(8)]
                for e4 in range(32):
                    b_ = trot.next()
                    for sl in range(4):
                        e2_ = e4 * 4 + sl
                        mm(b_[:, sl * 128:(sl + 1) * 128], R2[:, e2_, :], ident_b.ap, True, True, r2all + [ident_b], [b_])
                    cp("act", RT[:, :, e4 * 4:(e4 + 1) * 4], b_.ap.rearrange("p (e t) -> p t e", e=4), [b_], [(RT, e4)])
                rtall = [(RT, e4) for e4 in range(32)]
                for t4 in range(32):
                    b_ = trot.next()
                    for sl in range(4):
                        t_ = t4 * 4 + sl
                        mm(b_[:, sl * 128:(sl + 1) * 128], RT[:, t_, :], Pm[:, t_, :], True, True, rtall + [Pm], [b_])
                    cp("act", WTs[:, :, t4 * 4:(t4 + 1) * 4], b_.ap.rearrange("p (t e) -> p e t", t=4), [b_], [(WTs, t4)])
                wtall = [(WTs, t4) for t4 in range(32)]
                for q4 in range(4):
                    P.dma("pool", WT[q4 * 32:(q4 + 1) * 32, :, tc_].rearrange("a p t -> p a t"), WTs[:, q4 * 32:(q4 + 1) * 32, :], reads=wtall, writes=["WT"])
            P.barrier()
            A.reset(mk)
            if upto < 9:
                return
            h2T = A.alloc("h2T", [16, TOWN], BF16)
            P.dma("sp", h2T.ap, h2s, writes=[h2T])
            P.barrier()
            ust = Rot([A.alloc("ust", [16, 128]) for _ in range(4)])
            ub = Rot([A.alloc("ub", [16, 128], BF16) for _ in range(3)])
            wtb = Rot([A.alloc("wtb", [TOWN], BF16) for _ in range(3)])
            gl = Rot([A.alloc("gl", [512], BF16) for _ in range(3)])
            atb = Rot([A.alloc("atb", [TOWN], BF16) for _ in range(3)])
            uv = uT.rearrange("(kc p) e -> p kc e", p=128)
            pssets = [psb[0:4], psb[4:8]]
            for e1 in range(128):
                us = ust.next(); u_ = ub.next(); wt_ = wtb.next(); at_ = atb.next()
                P.dma("sp", us.ap, uv[:, :, e1 * 128:(e1 + 1) * 128], writes=[us])
                P.dma("sp", wt_.ap, WT[e1], writes=[wt_])
                cast(u_.ap, us.ap, [us], [u_])
                bset = pssets[e1 % 2]
                for kc in range(16):
                    for g in range(4):
                        qc = slice(g * 512, (g + 1) * 512)
                        mm(bset[g].ap, u_[:, kc, :], h2T[:, kc, qc], kc == 0, kc == 15, [u_, (h2T, kc, g)], [bset[g]])
                for g in range(4):
                    qc = slice(g * 512, (g + 1) * 512)
                    g_ = gl.next()
                    act(g_.ap, bset[g].ap, AF.Gelu, [bset[g]], [g_])
                    tt("dve", at_[:, qc], g_.ap, wt_[:, qc], ALU.mult, [g_, wt_], [(at_, g)])
                P.dma("pool", AT[e1], at_.ap, reads=[(at_, g) for g in range(4)], writes=["AT"])
            P.barrier()
            A.reset(mk)
            if upto < 10:
                return
            accb = A.alloc("accb", [16, TOWN])
            pssets = [psb[0:4], psb[4:8]]
            mkc = A.mark()
            vst = Rot([A.alloc("vst", [D]) for _ in range(1)])
            G = 4
            vbs = Rot([A.alloc("vb", [G, D], BF16) for _ in range(2)])
            abs_ = Rot([A.alloc("ab", [G, TOWN], BF16) for _ in range(2)])
            for eg in range(128 // G):
                vb = vbs.next(); ab = abs_.next()
                for l in range(G):
                    e1 = eg * G + l
                    vs = vst.next()
                    P.dma("sp", vs.ap, v_tab[e1 * 128:(e1 + 1) * 128, :], writes=[vs])
                    cast(vb[:, l, :], vs.ap, [vs], [(vb, l)])
                    P.dma("sp", ab[:, l, :], AT[e1], writes=[(ab, l)])
                for dc in range(16):
                    bset = pssets[dc % 2]
                    for l in range(G):
                        for g in range(4):
                            qc = slice(g * 512, (g + 1) * 512)
                            mm(bset[g].ap, vb[:, l, dc * 128:(dc + 1) * 128], ab[:, l, qc], l == 0, l == G - 1, [(vb, l), (ab, l)], [bset[g]])
                    for g in range(4):
                        qc = slice(g * 512, (g + 1) * 512)
                        if eg == 0:
                            cp("dve", accb[:, dc, qc], bset[g].ap, [bset[g]], [(accb, dc, g)])
                        else:
                            tt("dve", accb[:, dc, qc], accb[:, dc, qc], bset[g].ap, ALU.add, [(accb, dc, g), bset[g]], [(accb, dc, g)])
            if debug:
                P.dma("pool", dbg_peer, accb.ap, reads=[(accb, dc, g) for dc in range(16) for g in range(4)], writes=["dbg_peer"])
            P.barrier()
            A.reset(mkc)
            allacc = []
            xs = Rot([A.alloc("xs", [512]) for _ in range(3)])
            sqs = Rot([A.alloc("fsq", [512]) for _ in range(2)])
            r1 = A.alloc("fr1", [512]); rstd = A.alloc("frstd", [512])
            tms = Rot([A.alloc("ftm", [512]) for _ in range(3)])
            ssq = psb[7]
            for g in range(4):
                qc = slice(g * 512, (g + 1) * 512)
                for dc in range(16):
                    xk = xs.next()
                    P.dma("sp", xk.ap, x1T[dc * 128:(dc + 1) * 128, qc], writes=[xk])
                    stt(accb[:, dc, qc], accb[:, dc, qc], GT2[:, dc:dc + 1], xk.ap, ALU.mult, ALU.add, [(accb, dc, g), xk, "pers"], [(accb, dc, g)])
                    sq = sqs.next()
                    act(sq.ap, accb[:, dc, qc], AF.Square, [(accb, dc, g)], [sq])
                    mm(ssq.ap, ones_f.ap, sq.ap, dc == 0, dc == 15, [sq, ones_f], [ssq])
                act(r1.ap, ssq.ap, AF.Sqrt, [ssq], [r1], scale=1.0 / D, bias=EPS)
                P.op("dve", lambda e, o=rstd.ap, i=r1.ap: e.reciprocal(out=o, in_=i), reads=[r1], writes=[rstd])
                for dc in range(16):
                    tm = tms.next()
                    tt("dve", tm.ap, accb[:, dc, qc], rstd.ap, ALU.mult, [(accb, dc, g), rstd], [tm])
                    act(tm.ap, tm.ap, AF.Identity, [tm, "pers"], [tm], scale=GF[:, dc:dc + 1])
                    P.dma("pool", outT[dc * 128:(dc + 1) * 128, qc], tm.ap, reads=[tm], writes=["outT"])
            P.barrier()

        if upto >= 7:
            peer()
        P.barrier()
        P.emit()
    return nc
_PROG_CACHE = {}


def _own_idx(p):
    return np.concatenate([np.arange((2 * i + p) * 128, (2 * i + p + 1) * 128) for i in range(16)])


def _masks(p):
    j = np.arange(8)[None, :, None, None]
    s = np.arange(128)[:, None, None, None]
    il = np.arange(4)[None, None, :, None]
    r = np.arange(128)[None, None, None, :]
    mla = (2 * j + s // 64) <= (2 * (2 * il + p) + r // 64)
    sb = (128 * j + s) < (128 * (2 * il + p) + r)
    return (np.ascontiguousarray(mla.reshape(128, 8, 512)).astype(np.float32),
            np.ascontiguousarray(sb.reshape(128, 8, 512)).astype(np.float32))


def _col(v, n):
    return np.ascontiguousarray(np.asarray(v, np.float32).reshape(n, 128).T)


def make_in_maps(inputs, big_tabs=True, cores=range(8)):
    f = lambda k: np.asarray(inputs[k])
    x = f("x").astype(np.float32, copy=False)
    c = f("c"); positions = f("positions").astype(np.int32, copy=False)
    w_in = f("w_in")[0]
    cq, ckv, kr, sbq, sbk, sbv_, ga, gb = np.split(w_in, np.cumsum([512, 256, 64, 1024, 1024, 1024, 2048, 2048])[:-1].tolist(), axis=1)
    kr_sw = np.concatenate([kr[:, 32:], kr[:, :32]], axis=1)
    w_kv = np.ascontiguousarray(np.concatenate([ckv, kr, kr_sw, sbk, sbv_], axis=1))
    w_q1 = np.ascontiguousarray(np.concatenate([cq, sbq, ga, gb], axis=1))
    wuq = f("mla_w_uq")[0].reshape(512, 8, 192)
    w_uq_n = np.ascontiguousarray(wuq[:, :, :128].reshape(512, 1024))
    rope = wuq[:, :, 128:]
    w_uq_r = np.ascontiguousarray(rope.reshape(512, 512))
    w_uq_rs = np.ascontiguousarray(np.concatenate([rope[:, :, 32:], rope[:, :, :32]], axis=2).reshape(512, 512))
    wukv = f("mla_w_ukv")[0].reshape(256, 8, 256)
    w_ukv_k = np.ascontiguousarray(wukv[:, :, :128].reshape(256, 1024))
    w_ukv_v = np.ascontiguousarray(wukv[:, :, 128:].reshape(256, 1024))
    sk = f("peer_sub_keys")[0]
    k1T = np.ascontiguousarray(sk[:, 0].transpose(2, 0, 1))
    k2T = np.ascontiguousarray(sk[:, 1].transpose(2, 0, 1))
    if big_tabs:
        uT = np.ascontiguousarray(f("peer_u")[0].T)
        v_tab = np.ascontiguousarray(f("peer_v")[0])
    else:
        uT = np.zeros((128, 128), np.float32); v_tab = np.zeros((128, 128), np.float32)
    inv_freq = (np.float32(10000.0) ** (-np.arange(0, 64, 2, dtype=np.float32) / np.float32(64))).astype(np.float32)
    invf2 = np.concatenate([inv_freq, inv_freq])[:, None].astype(np.float32)
    sgn = np.concatenate([-np.ones(32, np.float32), np.ones(32, np.float32)])[:, None]
    ident = np.eye(128, dtype=np.float32)
    tmat = np.tril(np.ones((128, 128), np.float32), 0)
    shared = dict(
        ada_w=np.ascontiguousarray(f("ada_w")[0]), ada_b_col=_col(f("ada_b")[0], 96),
        g1_col=_col(f("norm1_g")[0], 16), g2_col=_col(f("norm2_g")[0], 16), gf_col=_col(f("final_norm_g"), 16),
        w_kv=w_kv, w_q1=w_q1, qn_g=_col(f("mla_q_norm_g")[0], 4), kvn_g=_col(f("mla_kv_norm_g")[0], 2),
        w_uq_n=w_uq_n, w_uq_r=w_uq_r, w_uq_rs=w_uq_rs, w_ukv_k=w_ukv_k, w_ukv_v=w_ukv_v,
        w_bm=np.ascontiguousarray(f("w_branch_mla")[0]), w_bs=np.ascontiguousarray(f("w_branch_sb")[0]),
        w_out=np.ascontiguousarray(f("w_out")[0]), w_pq=np.ascontiguousarray(f("peer_w_q")[0]),
        k1T=k1T, k2T=k2T, umat=np.ascontiguousarray(np.triu(np.ones((128, 128), np.float32), 1)), iota=np.ascontiguousarray(np.broadcast_to(np.arange(128, dtype=np.float32)[None, :], (128, 128))), uT=uT, v_tab=v_tab, invf2=invf2, sgn=sgn, ident=ident, tmat=tmat)
    masks = [_masks(0), _masks(1)]
    maps = []
    for core in cores:
        b, p = core // 2, core % 2
        idx = _own_idx(p)
        m = dict(shared)
        m["xT_nat"] = np.ascontiguousarray(x[b].T)
        m["xT_own"] = np.ascontiguousarray(x[b][idx].T)
        m["pos_nat"] = np.ascontiguousarray(positions[b][None, :])
        m["pos_own"] = np.ascontiguousarray(positions[b][idx][None, :])
        m["c_col"] = _col(c[b], 16)
        m["mask_mla"], m["mask_sb"] = masks[p]
        maps.append(m)
    return maps


def kernel(**inputs):
    if "nc" not in _PROG_CACHE:
        _PROG_CACHE["nc"] = build_program()
    nc = _PROG_CACHE["nc"]
    in_maps = make_in_maps(inputs)
    res = run_bass_kernel_spmd(nc, in_maps, core_ids=list(range(8)))
    out = np.empty((4, SEQ, D), np.float32)
    for core in range(8):
        b, p = core // 2, core % 2
        out[b, _own_idx(p), :] = np.asarray(res.results[core]["outT"]).T
    return out
```
